# Optimizing a Trainium2 kernel written in Bass

```python
import math
import jax, jax.numpy as jnp
from jax import lax
import numpy as np

D_MODEL = 1024
BATCH = 1
SEQ = 16384
DEPTH = 1

HEAD_DIM = 64
A_Q_HEADS = 8
A_KV_HEADS = 2
A_GROUP = A_Q_HEADS // A_KV_HEADS
B_HEADS = 4
B_V_DIM = 2 * HEAD_DIM
Q_BLOCK = 128
GRID_W = 64
AXIAL_THETA = 10000.0
AXIAL_HALF = HEAD_DIM // 2
ROPE_THETA = 500000.0
ROPE_DIMS = HEAD_DIM // 4
NORM_EPS = 1e-6
SUBLN_EPS = 1e-5

A_Q_W = A_Q_HEADS * HEAD_DIM
A_KV_W = A_KV_HEADS * HEAD_DIM
B_QK_W = 2 * B_HEADS * HEAD_DIM
B_V_W = B_HEADS * B_V_DIM
IN_SPLITS = [A_Q_W, A_KV_W, A_KV_W, B_QK_W, B_QK_W, B_V_W]
IN_COLS = sum(IN_SPLITS)
MIX_WIDTH = A_Q_W + B_V_W

PEER_HEADS = 8
PEER_KEYS = 128
PEER_EXPERTS = PEER_KEYS * PEER_KEYS
PEER_QUERY_DIM = 256
PEER_HALF = PEER_QUERY_DIM // 2
PEER_TOPK = 16
TOKEN_CHUNK = 128

kernel_name = "hybrid_gqa_diffattn_peer_encoder"


def _rms_norm(x, g, eps=NORM_EPS):
    xf = x.astype(jnp.float32)
    y = xf * lax.rsqrt(jnp.mean(xf * xf, axis=-1, keepdims=True) + eps)
    return (y * g.astype(jnp.float32)).astype(x.dtype)


def _rotate(x, ang):
    xf = x.astype(jnp.float32)
    x1, x2 = jnp.split(xf, 2, axis=-1)
    c, s = jnp.cos(ang), jnp.sin(ang)
    return jnp.concatenate([x1 * c - x2 * s, x2 * c + x1 * s], axis=-1).astype(x.dtype)


def _axial_rope(x, row_ang, col_ang):
    return jnp.concatenate([_rotate(x[..., :AXIAL_HALF], row_ang),
                            _rotate(x[..., AXIAL_HALF:], col_ang)], axis=-1)


def _partial_rope(x, ang):
    return jnp.concatenate([_rotate(x[..., :ROPE_DIMS], ang), x[..., ROPE_DIMS:]], axis=-1)


def _gqa_attention(q, k, v):
    B, Hk, G, S, d = q.shape
    nb = S // Q_BLOCK
    scale = d ** -0.5
    qb = jnp.moveaxis(q.reshape(B, Hk, G, nb, Q_BLOCK, d), 3, 0)

    def one(qi):
        s = jnp.einsum('bhgqd,bhkd->bhgqk', qi, k).astype(jnp.float32) * scale
        p = jax.nn.softmax(s, axis=-1)
        return jnp.einsum('bhgqk,bhkv->bhgqv', p.astype(v.dtype), v)

    o = lax.map(one, qb)
    return jnp.moveaxis(o, 0, 3).reshape(B, Hk, G, S, v.shape[-1])


def _diff_attention(q, k, v, lam):
    B, H, _, S, d = q.shape
    nb = S // Q_BLOCK
    scale = d ** -0.5
    qb = jnp.moveaxis(q.reshape(B, H, 2, nb, Q_BLOCK, d), 3, 0)

    def one(qi):
        s = jnp.einsum('bhcqd,bhckd->bhcqk', qi, k).astype(jnp.float32) * scale
        p = jax.nn.softmax(s, axis=-1)
        w = p[:, :, 0] - lam * p[:, :, 1]
        return jnp.einsum('bhqk,bhkv->bhqv', w.astype(v.dtype), v)

    o = lax.map(one, qb)
    return jnp.moveaxis(o, 0, 2).reshape(B, H, S, v.shape[-1])


def _peer(xn, w_query, sub_keys, expert_u, expert_v):
    B, S, D = xn.shape
    xc_all = xn.reshape(B * S // TOKEN_CHUNK, TOKEN_CHUNK, D)

    def one(xc):
        C = xc.shape[0]
        q = (xc @ w_query).reshape(C, PEER_HEADS, 2, PEER_HALF)
        sc = jnp.einsum('chpd,hpnd->chpn', q, sub_keys).astype(jnp.float32)
        v1, i1 = lax.top_k(sc[:, :, 0], PEER_TOPK)
        v2, i2 = lax.top_k(sc[:, :, 1], PEER_TOPK)
        cand = (v1[..., :, None] + v2[..., None, :]).reshape(C, PEER_HEADS, PEER_TOPK * PEER_TOPK)
        cand_idx = (i1[..., :, None] * PEER_KEYS + i2[..., None, :]).reshape(C, PEER_HEADS, PEER_TOPK * PEER_TOPK)
        top, pos = lax.top_k(cand, PEER_TOPK)
        idx = jnp.take_along_axis(cand_idx, pos, axis=-1)
        g = jax.nn.softmax(top, axis=-1)
        u = expert_u[idx]
        v = expert_v[idx]
        a = jax.nn.gelu(jnp.einsum('cd,chkd->chk', xc, u), approximate=False)
        return jnp.einsum('chk,chkd->cd', (g * a.astype(jnp.float32)).astype(v.dtype), v)

    return lax.map(one, xc_all).reshape(B, S, D)


def setup_inputs(seed: int = 0) -> dict:
    key = jax.random.key(seed)
    ks = jax.random.split(key, 20)
    f32 = jnp.float32
    L = DEPTH

    def nrm(k, shape, scale):
        return jax.random.normal(k, shape, f32) * scale

    def gain(k, shape):
        return 1.0 + 0.02 * jax.random.normal(k, shape, f32)

    return {
        "x": jax.random.normal(ks[0], (BATCH, SEQ, D_MODEL), f32),
        "norm_attn_g": gain(ks[1], (L, D_MODEL)),
        "w_in": nrm(ks[2], (L, D_MODEL, IN_COLS), D_MODEL ** -0.5),
        "q_norm_g": gain(ks[3], (L, HEAD_DIM)),
        "k_norm_g": gain(ks[4], (L, HEAD_DIM)),
        "lambda_q1": nrm(ks[5], (L, HEAD_DIM), 0.1),
        "lambda_k1": nrm(ks[6], (L, HEAD_DIM), 0.1),
        "lambda_q2": nrm(ks[7], (L, HEAD_DIM), 0.1),
        "lambda_k2": nrm(ks[8], (L, HEAD_DIM), 0.1),
        "subln_g": gain(ks[9], (L, B_V_DIM)),
        "w_out": nrm(ks[10], (L, MIX_WIDTH, D_MODEL), MIX_WIDTH ** -0.5),
        "norm_ffn_g": gain(ks[11], (L, D_MODEL)),
        "w_query": nrm(ks[12], (L, D_MODEL, PEER_HEADS * PEER_QUERY_DIM), D_MODEL ** -0.5),
        "sub_keys": nrm(ks[13], (L, PEER_HEADS, 2, PEER_KEYS, PEER_HALF), PEER_HALF ** -0.5),
        "expert_u": nrm(ks[14], (L, PEER_EXPERTS, D_MODEL), D_MODEL ** -0.5),
        "expert_v": nrm(ks[15], (L, PEER_EXPERTS, D_MODEL), 0.25),
        "norm_final_g": gain(ks[16], (D_MODEL,)),
    }


def reference(x, norm_attn_g, w_in, q_norm_g, k_norm_g, lambda_q1, lambda_k1,
              lambda_q2, lambda_k2, subln_g, w_out, norm_ffn_g, w_query, sub_keys,
              expert_u, expert_v, norm_final_g):
    B, S, _ = x.shape
    rows = S // GRID_W
    row = jnp.repeat(jnp.arange(rows, dtype=jnp.float32), GRID_W)
    col = jnp.tile(jnp.arange(GRID_W, dtype=jnp.float32), rows)
    pos = jnp.arange(S, dtype=jnp.float32)
    inv_ax = AXIAL_THETA ** (-jnp.arange(0, AXIAL_HALF, 2, dtype=jnp.float32) / AXIAL_HALF)
    inv_p = ROPE_THETA ** (-jnp.arange(0, ROPE_DIMS, 2, dtype=jnp.float32) / ROPE_DIMS)
    row_ang = row[:, None] * inv_ax[None, :]
    col_ang = col[:, None] * inv_ax[None, :]
    pos_ang = pos[:, None] * inv_p[None, :]
    split_pts = [int(v) for v in np.cumsum(IN_SPLITS)[:-1]]

    h = x
    for l in range(DEPTH):
        lambda_init = 0.8 - 0.6 * math.exp(-0.3 * l)
        xn = _rms_norm(h, norm_attn_g[l])
        proj = xn @ w_in[l]
        qa, ka, va, qb, kb, vb = jnp.split(proj, split_pts, axis=-1)

        qa = qa.reshape(B, S, A_Q_HEADS, HEAD_DIM).transpose(0, 2, 1, 3)
        ka = ka.reshape(B, S, A_KV_HEADS, HEAD_DIM).transpose(0, 2, 1, 3)
        va = va.reshape(B, S, A_KV_HEADS, HEAD_DIM).transpose(0, 2, 1, 3)
        qa = _axial_rope(_rms_norm(qa, q_norm_g[l]), row_ang, col_ang)
        ka = _axial_rope(_rms_norm(ka, k_norm_g[l]), row_ang, col_ang)
        qa = qa.reshape(B, A_KV_HEADS, A_GROUP, S, HEAD_DIM)
        oa = _gqa_attention(qa, ka, va).reshape(B, A_Q_HEADS, S, HEAD_DIM)
        oa = oa.transpose(0, 2, 1, 3).reshape(B, S, A_Q_W)

        qb = qb.reshape(B, S, B_HEADS, 2, HEAD_DIM).transpose(0, 2, 3, 1, 4)
        kb = kb.reshape(B, S, B_HEADS, 2, HEAD_DIM).transpose(0, 2, 3, 1, 4)
        vb = vb.reshape(B, S, B_HEADS, B_V_DIM).transpose(0, 2, 1, 3)
        qb = _partial_rope(qb, pos_ang)
        kb = _partial_rope(kb, pos_ang)
        lam = (jnp.exp(jnp.sum(lambda_q1[l].astype(jnp.float32) * lambda_k1[l].astype(jnp.float32)))
               - jnp.exp(jnp.sum(lambda_q2[l].astype(jnp.float32) * lambda_k2[l].astype(jnp.float32)))
               + lambda_init)
        ob = _diff_attention(qb, kb, vb, lam)
        ob = _rms_norm(ob, subln_g[l], SUBLN_EPS) * (1.0 - lambda_init)
        ob = ob.transpose(0, 2, 1, 3).reshape(B, S, B_V_W)

        h = h + jnp.concatenate([oa, ob], axis=-1) @ w_out[l]

        h = h + _peer(_rms_norm(h, norm_ffn_g[l]), w_query[l], sub_keys[l],
                      expert_u[l], expert_v[l])

    return _rms_norm(h, norm_final_g)
```

```python
import math
from contextlib import ExitStack

import numpy as np
import concourse.bass as bass
import concourse.mybir as mybir
from concourse.bass_utils import run_bass_kernel_spmd

F32 = mybir.dt.float32
BF16 = mybir.dt.bfloat16
ALU = mybir.AluOpType
AF = mybir.ActivationFunctionType
AX = mybir.AxisListType

NCORE = 8
D = 1024
NORM_EPS = 1e-6
SUBLN_EPS = 1e-5
LAMBDA_INIT = 0.8 - 0.6 * math.exp(-0.3 * 0)
ENGS = ["pe", "act", "dve", "pool", "sp"]


class Buf:
    __slots__ = ("name", "w", "r", "dcnt", "sem", "uses_dma", "excl")

    def __init__(self, name, excl=False):
        self.name = name
        self.excl = excl
        self.w = None
        self.r = {}
        self.dcnt = 0
        self.sem = None
        self.uses_dma = False


class Op:
    __slots__ = ("eng", "fn", "deps", "tok", "dma", "waits", "inc")

    def __init__(self, eng, fn, deps, tok, dma):
        self.eng = eng
        self.fn = fn
        self.deps = deps
        self.tok = tok
        self.dma = dma
        self.waits = []
        self.inc = False


class Rec:
    def __init__(self):
        self.ops = []
        self.cnt = {e: 0 for e in ENGS}
        self.dma_bufs = []
        self.pending = {e: set() for e in ENGS}

    def _deps(self, eng, reads, writes):
        deps = set()
        for b in reads:
            if b.w is not None:
                if not (b.w[0] == "e" and b.w[1] == eng and eng == "pe"):
                    deps.add(b.w)
            if b.excl:
                for t in b.r.values():
                    if not (t[0] == "e" and t[1] == eng):
                        deps.add(t)
        for b in writes:
            if b.w is not None and not (b.w[0] == "e" and b.w[1] == eng):
                deps.add(b.w)
            for t in b.r.values():
                if not (t[0] == "e" and t[1] == eng):
                    deps.add(t)
        if self.pending[eng]:
            deps |= self.pending[eng]
            self.pending[eng] = set()
        return deps

    def op(self, eng, fn, reads=(), writes=()):
        deps = self._deps(eng, reads, writes)
        self.cnt[eng] += 1
        tok = ("e", eng, self.cnt[eng])
        self.ops.append(Op(eng, fn, deps, tok, None))
        for b in writes:
            b.w = tok
            b.r = {}
        for b in reads:
            b.r[eng] = tok
        return tok

    def dma(self, eng, fn, anchor, reads=(), writes=()):
        deps = self._deps(eng, reads, writes)
        if not anchor.uses_dma:
            anchor.uses_dma = True
            self.dma_bufs.append(anchor)
        anchor.dcnt += 16
        tok = ("d", anchor, anchor.dcnt)
        self.ops.append(Op(eng, fn, deps, tok, anchor))
        for b in writes:
            b.w = tok
            b.r = {}
        for b in reads:
            b.r[("d", anchor)] = tok
        return tok

    def barrier(self):
        toks = set()
        for e in ENGS:
            if e != "sp" and self.cnt[e] > 0:
                toks.add(("e", e, self.cnt[e]))
        for b in self.dma_bufs:
            toks.add(("d", b, b.dcnt))
        for e in ENGS:
            self.pending[e] |= {t for t in toks if not (t[0] == "e" and t[1] == e)}

    def finish(self, nc, stack):
        self.barrier()
        self.ops.append(Op("sp", None, self.pending["sp"], None, None))
        self.pending["sp"] = set()
        needed = set()
        waited = {e: {} for e in ENGS}
        for op in self.ops:
            w = waited[op.eng]
            for d in sorted(op.deps, key=lambda t: (t[0], str(t[1] if t[0] == "e" else t[1].name), t[2])):
                key = d[1]
                if w.get(key, 0) < d[2]:
                    w[key] = d[2]
            per = {}
            for d in op.deps:
                key = d[1]
                if key not in per or per[key][2] < d[2]:
                    per[key] = d
            op.waits = list(per.values())
        waited = {e: {} for e in ENGS}
        for op in self.ops:
            w = waited[op.eng]
            keep = []
            for d in op.waits:
                key = d[1]
                if w.get(key, 0) < d[2]:
                    w[key] = d[2]
                    keep.append(d)
                    if d[0] == "e":
                        needed.add((d[1], d[2]))
            op.waits = keep
        incval = {}
        c = {e: 0 for e in ENGS}
        for op in self.ops:
            if op.tok is not None and op.tok[0] == "e" and (op.tok[1], op.tok[2]) in needed:
                c[op.eng] += 1
                incval[(op.tok[1], op.tok[2])] = c[op.eng]
                op.inc = True
        esem = {e: stack.enter_context(nc.semaphore("es_" + e)) for e in ENGS if e != "sp"}
        for b in self.dma_bufs:
            b.sem = stack.enter_context(nc.semaphore("ds_" + b.name))
        per_eng = {e: [o for o in self.ops if o.eng == e] for e in ENGS}

        def emit(engobj, ename):
            for op in per_eng[ename]:
                for d in op.waits:
                    if d[0] == "e":
                        engobj.wait_ge(esem[d[1]], incval[(d[1], d[2])])
                    else:
                        engobj.wait_ge(d[1].sem, d[2])
                if op.fn is None:
                    continue
                ins = op.fn(engobj)
                if op.dma is not None:
                    ins.then_inc(op.dma.sem, 16)
                elif op.inc:
                    ins.then_inc(esem[ename], 1)

        with nc.Block() as block:
            @block.sync
            def _(e):
                emit(e, "sp")

            @block.tensor
            def _(e):
                emit(e, "pe")

            @block.scalar
            def _(e):
                emit(e, "act")

            @block.vector
            def _(e):
                emit(e, "dve")

            @block.gpsimd
            def _(e):
                emit(e, "pool")
        return {e: c[e] for e in ENGS}


class Arena:
    def __init__(self, ap, nwords):
        self.ap = ap
        self.n = nwords
        self.off = 0
        self.peak = 0

    def alloc(self, shape, dtype, top=False):
        n = int(np.prod(shape))
        words = n if dtype == F32 else (n + 1) // 2
        if top:
            self.n -= words
            a = self.ap[:, self.n:self.n + words]
        else:
            a = self.ap[:, self.off:self.off + words]
            self.off += words
        self.peak = max(self.peak, self.off)
        assert self.off <= self.n, ("arena overflow", self.off, self.n)
        if dtype != F32:
            a = a.bitcast(dtype)
        if len(shape) == 1:
            return a
        if len(shape) == 2:
            return a.rearrange("p (a b) -> p a b", b=shape[1])
        if len(shape) == 3:
            return a.rearrange("p (a b c) -> p a b c", b=shape[1], c=shape[2])
        if len(shape) == 4:
            return a.rearrange("p (a b c d) -> p a b c d", b=shape[1], c=shape[2], d=shape[3])
        raise ValueError(shape)


def build(S, with_peer=True):
    TQ = S // NCORE
    QB = min(512, TQ)
    NQB = TQ // QB
    NQC = QB // 128
    NQI = TQ // 128
    KB = 512
    NKB = S // KB
    NE = 16384
    nc = bass.Bass("TRN2", target_bir_lowering=False)
    R = Rec()

    def din(name, shape):
        return nc.dram_tensor(name, list(shape), F32, kind="ExternalInput").ap()

    d_xT = din("xT", [128, 8, S])
    d_xTo = din("xTo", [128, 8, TQ])
    d_xo = din("xo", [TQ, D])
    d_wA = din("wA", [128, 8, 768])
    d_wB = din("wB", [128, 8, 1536])
    d_wout = din("wout", [128, 8, D])
    d_cst = din("cst", [128, 5, 128])
    d_tabA = din("tabA", [128, 2, S])
    d_tabB = din("tabB", [128, 2, S])
    d_tabAo = din("tabAo", [128, 2, TQ])
    d_tabBo = din("tabBo", [128, 2, TQ])
    d_vec = din("vec", [128, 8 + 8 + 2])
    d_lam = din("lam", [128, 4, 64])
    d_sub = din("subln", [128, 128])
    d_gfin = din("gfin", [128, D])
    d_gffn = din("gffn", [128, D])
    if with_peer:
        d_wq = din("wq", [128, 8, 2048])
        d_skT = din("skT", [128, 16, 128])
        d_uT = din("uT", [128, 8, NE])
        d_ev = din("ev", [NE, D])
        d_hs = nc.dram_tensor("hscr", [TQ, D], F32, kind="Internal").ap()
    d_out = nc.dram_tensor("out", [TQ, D], F32, kind="ExternalOutput").ap()

    stack = ExitStack()
    ARENA_WORDS = 53200
    arena_t = stack.enter_context(nc.sbuf_tensor("arena", [128, ARENA_WORDS], F32))
    A = Arena(arena_t[:], ARENA_WORDS)
    ps_S = [stack.enter_context(nc.psum_tensor("psS%d" % i, [128, 512], F32)) for i in range(2)]
    ps_O = [stack.enter_context(nc.psum_tensor("psO%d" % i, [128, 1024], F32)) for i in range(2)]
    ps_P = [stack.enter_context(nc.psum_tensor("psP%d" % i, [128, 512], F32)) for i in range(2)]
    bS = [Buf("psS%d" % i, True) for i in range(2)]
    bO = [Buf("psO%d" % i, True) for i in range(2)]
    bP = [Buf("psP%d" % i, True) for i in range(2)]
    pp_i = [0]

    def next_pp():
        i = pp_i[0] % 2
        pp_i[0] += 1
        return ps_P[i][:], bP[i]

    cst_f = A.alloc([5, 128], F32)
    cst = A.alloc([5, 128], BF16)
    vec = A.alloc([18], F32)
    lam4 = A.alloc([4, 64], F32)
    subg = A.alloc([128], F32)
    gfin = A.alloc([D], F32)
    small = A.alloc([64], F32)
    b_cst = Buf("cst")
    b_small = Buf("small")
    ident = cst[:, 0, :]
    ones_bf = cst[:, 1, :]
    onesblk = cst[:, 2, :]
    permA = cst[:, 3, :]
    permB = cst[:, 4, :]
    Otok = A.alloc([NQI, D], BF16, top=True)
    b_Otok = [Buf("Otok%d" % i) for i in range(NQI)]

    R.dma("sp", lambda e: e.dma_start(out=cst_f, in_=d_cst), b_cst, writes=[b_cst])
    R.dma("sp", lambda e: e.dma_start(out=vec, in_=d_vec), b_cst, writes=[b_cst])
    R.dma("sp", lambda e: e.dma_start(out=lam4, in_=d_lam), b_cst, writes=[b_cst])
    R.dma("sp", lambda e: e.dma_start(out=subg, in_=d_sub), b_cst, writes=[b_cst])
    R.dma("sp", lambda e: e.dma_start(out=gfin, in_=d_gfin), b_cst, writes=[b_cst])
    b_cbf = Buf("cbf")
    R.op("dve", lambda e: e.tensor_copy(out=cst, in_=cst_f), reads=[b_cst], writes=[b_cbf])
    lt = A.alloc([2, 64], F32)
    R.op("dve", lambda e: e.tensor_tensor(out=lt[:, 0, :], in0=lam4[:, 0, :], in1=lam4[:, 1, :], op=ALU.mult),
         reads=[b_cst], writes=[b_small])
    R.op("dve", lambda e: e.tensor_tensor(out=lt[:, 1, :], in0=lam4[:, 2, :], in1=lam4[:, 3, :], op=ALU.mult),
         reads=[b_cst, b_small], writes=[b_small])
    R.op("dve", lambda e: e.reduce_sum(out=small[:, 2:4], in_=lt, axis=AX.X), reads=[b_small], writes=[b_small])
    R.op("act", lambda e: e.activation(out=small[:, 4:6], in_=small[:, 2:4], func=AF.Exp),
         reads=[b_small], writes=[b_small])
    R.op("dve", lambda e: e.tensor_tensor(out=small[:, 0:1], in0=small[:, 4:5], in1=small[:, 5:6], op=ALU.subtract),
         reads=[b_small], writes=[b_small])
    R.op("dve", lambda e: e.tensor_scalar(out=small[:, 0:1], in0=small[:, 0:1], scalar1=float(LAMBDA_INIT),
                                          scalar2=None, op0=ALU.add), reads=[b_small], writes=[b_small])
    R.op("dve", lambda e: e.tensor_scalar(out=subg, in0=subg, scalar1=float(1.0 - LAMBDA_INIT), scalar2=None,
                                          op0=ALU.mult), reads=[b_cst], writes=[b_cst])
    lam = small[:, 0:1]

    def rsqrt_mean(eng, out, in_, n, eps, reads, writes):
        R.op(eng, lambda e: e.tensor_scalar(out=out, in0=in_, scalar1=1.0 / n, scalar2=float(eps),
                                            op0=ALU.mult, op1=ALU.add), reads=reads, writes=writes)
        R.op("act", lambda e: e.activation(out=out, in_=out, func=AF.Ln), reads=writes, writes=writes)
        R.op("act", lambda e: e.activation(out=out, in_=out, func=AF.Exp, scale=-0.5), reads=writes, writes=writes)

    phase_mark = A.off

    def attn_phase(tag, d_w, ncol_q, nkc, vdim, ngroups, headnorm, d_tab, d_tabo, perm, maps, ocol0, diff):
        A.off = phase_mark
        R.barrier()
        ncol = ncol_q + nkc * 128 + ngroups * vdim
        kcol0 = ncol_q
        vcol0 = ncol_q + nkc * 128
        nqch = ncol_q // 128
        vd1 = vdim + 1
        nmap = len(maps)
        wbf = A.alloc([8, ncol], BF16)
        b_w = Buf(tag + "w")
        QT = A.alloc([nqch, TQ], BF16)
        b_QT = Buf(tag + "QT")
        acc = A.alloc([nmap, NQI, vd1], F32)
        b_acc = Buf(tag + "acc")
        xTb = A.alloc([8, 256], F32)
        b_xT = Buf(tag + "xT")
        sq = A.alloc([8, 256], BF16)
        b_sq = Buf(tag + "sq")
        rstd = A.alloc([256], F32)
        b_rstd = Buf(tag + "rstd")
        xn = A.alloc([8, 512], BF16)
        b_xn = Buf(tag + "xn")
        tab = A.alloc([2, 512], F32)
        b_tab = Buf(tag + "tab")
        KT = [A.alloc([nkc, 512], BF16) for _ in range(2)]
        b_KT = [Buf(tag + "KT%d" % i) for i in range(2)]
        Vt_flat = [A.alloc([4 * ngroups * (vdim + 2)], BF16) for _ in range(2)]
        Vt = [v.rearrange("p (t g v) -> p t g v", g=ngroups, v=vdim + 2) for v in Vt_flat]
        b_Vt = [Buf(tag + "Vt%d" % i) for i in range(2)]
        Pb = [A.alloc([512], BF16) for _ in range(3)]
        b_Pb = [Buf(tag + "P%d" % i) for i in range(3)]
        t_sq = A.alloc([512], BF16)
        t_rs = A.alloc([512], F32)
        t_n = A.alloc([512], BF16)
        t_1 = A.alloc([512], F32)
        t_2 = A.alloc([512], F32)
        b_tsq, b_trs, b_tn, b_t1, b_t2 = (Buf(tag + n) for n in ("tsq", "trs", "tn", "t1", "t2"))
        wst, b_wst = t_2, b_t2

        for c in range(8):
            for c0 in range(0, ncol, 512):
                cw = min(512, ncol - c0)
                R.dma("sp", lambda e, c=c, c0=c0, cw=cw: e.dma_start(out=wst[:, 0:cw], in_=d_w[:, c, c0:c0 + cw]),
                      b_wst, writes=[b_wst])
                R.op("dve", lambda e, c=c, c0=c0, cw=cw: e.tensor_scalar(
                    out=wbf[:, c, c0:c0 + cw], in0=wst[:, 0:cw], scalar1=vec[:, c:c + 1], scalar2=None,
                    op0=ALU.mult), reads=[b_wst, b_cst], writes=[b_w])
        for i in range(2):
            R.op("dve", lambda e, i=i: e.memset(Vt_flat[i], 1.0), writes=[b_Vt[i]])

        def norm_unit(src, dst):
            R.dma("sp", lambda e: e.dma_start(out=xTb, in_=src), b_xT, writes=[b_xT])
            R.op("act", lambda e: e.activation(out=sq, in_=xTb, func=AF.Square), reads=[b_xT], writes=[b_sq])
            yield
            ps, bps = next_pp()
            for c in range(8):
                R.op("pe", lambda e, c=c: e.matmul(ps[:, 0:256], lhsT=ones_bf, rhs=sq[:, c, :], start=(c == 0),
                                                   stop=(c == 7)), reads=[b_sq, b_cbf], writes=[bps])
            rsqrt_mean("dve", rstd, ps[:, 0:256], float(D), NORM_EPS, [bps], [b_rstd])
            R.op("dve", lambda e: e.tensor_tensor(out=dst, in0=xTb,
                                                  in1=rstd.unsqueeze(1).to_broadcast([128, 8, 256]), op=ALU.mult),
                 reads=[b_xT, b_rstd], writes=[b_xn])
            yield

        def run(gen):
            for _ in gen:
                pass

        def load_tab(src):
            R.dma("sp", lambda e: e.dma_start(out=tab, in_=src), b_tab, writes=[b_tab])

        def proj_post(wcol, n, gcol, dst, b_dst):
            ps, bps = next_pp()
            for c in range(8):
                R.op("pe", lambda e, c=c: e.matmul(ps[:, 0:n], lhsT=wbf[:, c, wcol:wcol + 128], rhs=xn[:, c, 0:n],
                                                   start=(c == 0), stop=(c == 7)), reads=[b_w, b_xn], writes=[bps])
            if headnorm:
                R.op("act", lambda e: e.activation(out=t_sq[:, 0:n], in_=ps[:, 0:n], func=AF.Square),
                     reads=[bps], writes=[b_tsq])
                yield
                ps2, bps2 = next_pp()
                R.op("pe", lambda e: e.matmul(ps2[:, 0:n], lhsT=onesblk, rhs=t_sq[:, 0:n], start=True, stop=True),
                     reads=[b_tsq, b_cbf], writes=[bps2])
                rsqrt_mean("dve", t_rs[:, 0:n], ps2[:, 0:n], 64.0, NORM_EPS, [bps2], [b_trs])
                R.op("dve", lambda e: e.scalar_tensor_tensor(out=t_1[:, 0:n], in0=ps[:, 0:n],
                                                             scalar=vec[:, gcol:gcol + 1], in1=t_rs[:, 0:n],
                                                             op0=ALU.mult, op1=ALU.mult),
                     reads=[bps, b_trs, b_cst], writes=[b_t1])
                R.op("pool", lambda e: e.tensor_copy(out=t_n[:, 0:n], in_=t_1[:, 0:n]), reads=[b_t1], writes=[b_tn])
                yield
                ps3, bps3 = bps2 and (ps2, bps2)
                R.op("pe", lambda e: e.matmul(ps3[:, 0:n], lhsT=perm, rhs=t_n[:, 0:n], start=True, stop=True),
                     reads=[b_tn, b_cbf], writes=[bps3])
                R.op("pool", lambda e: e.tensor_tensor(out=t_1[:, 0:n], in0=t_1[:, 0:n], in1=tab[:, 0, 0:n],
                                                       op=ALU.mult), reads=[b_t1, b_tab], writes=[b_t1])
            else:
                R.op("act", lambda e: e.activation(out=t_n[:, 0:n], in_=ps[:, 0:n], func=AF.Copy),
                     reads=[bps], writes=[b_tn])
                yield
                ps3, bps3 = next_pp()
                R.op("pe", lambda e: e.matmul(ps3[:, 0:n], lhsT=perm, rhs=t_n[:, 0:n], start=True, stop=True),
                     reads=[b_tn, b_cbf], writes=[bps3])
                R.op("dve", lambda e: e.tensor_tensor(out=t_1[:, 0:n], in0=ps[:, 0:n], in1=tab[:, 0, 0:n],
                                                      op=ALU.mult), reads=[bps, b_tab], writes=[b_t1])
            R.op("dve", lambda e: e.tensor_tensor(out=t_2[:, 0:n], in0=ps3[:, 0:n], in1=tab[:, 1, 0:n], op=ALU.mult),
                 reads=[bps3, b_tab], writes=[b_t2])
            R.op("pool", lambda e: e.tensor_tensor(out=dst, in0=t_1[:, 0:n], in1=t_2[:, 0:n], op=ALU.add),
                 reads=[b_t1, b_t2], writes=[b_dst])
            yield

        for qb in range(NQB):
            for hf in range(QB // 256 if QB >= 256 else 1):
                w_ = min(256, QB)
                t0 = qb * QB + hf * w_
                if w_ == 256:
                    run(norm_unit(d_xTo[:, :, t0:t0 + 256], xn[:, :, hf * 256:(hf + 1) * 256]))
                else:
                    R.dma("sp", lambda e, t0=t0: e.dma_start(out=xTb[:, :, 0:128], in_=d_xTo[:, :, t0:t0 + 128]),
                          b_xT, writes=[b_xT])
                    R.op("act", lambda e: e.activation(out=sq[:, :, 0:128], in_=xTb[:, :, 0:128], func=AF.Square),
                         reads=[b_xT], writes=[b_sq])
                    ps, bps = next_pp()
                    for c in range(8):
                        R.op("pe", lambda e, c=c, ps=ps: e.matmul(ps[:, 0:128], lhsT=ones_bf, rhs=sq[:, c, 0:128],
                                                                  start=(c == 0), stop=(c == 7)),
                             reads=[b_sq, b_cbf], writes=[bps])
                    rsqrt_mean("dve", rstd[:, 0:128], ps[:, 0:128], float(D), NORM_EPS, [bps], [b_rstd])
                    R.op("dve", lambda e: e.tensor_tensor(
                        out=xn[:, :, 0:128], in0=xTb[:, :, 0:128],
                        in1=rstd[:, 0:128].unsqueeze(1).to_broadcast([128, 8, 128]), op=ALU.mult),
                         reads=[b_xT, b_rstd], writes=[b_xn])
            load_tab(d_tabo[:, :, qb * QB:(qb + 1) * QB]) if QB == 512 else R.dma(
                "sp", lambda e, qb=qb: e.dma_start(out=tab[:, :, 0:QB], in_=d_tabo[:, :, qb * QB:(qb + 1) * QB]),
                b_tab, writes=[b_tab])
            for j in range(nqch):
                run(proj_post(j * 128, QB, 16, QT[:, j, qb * QB:(qb + 1) * QB], b_QT))

        def inproj(kb):
            s = kb % 2
            for hf in range(2):
                t0 = kb * KB + hf * 256
                yield from norm_unit(d_xT[:, :, t0:t0 + 256], xn[:, :, hf * 256:(hf + 1) * 256])
            load_tab(d_tab[:, :, kb * KB:(kb + 1) * KB])
            for kc in range(nkc):
                yield from proj_post(kcol0 + kc * 128, 512, 17, KT[s][:, kc, :], b_KT[s])
            for tt in range(4):
                vw = ngroups * vdim
                for c0 in range(0, vw, 512):
                    cw = min(512, vw - c0)
                    ps, bps = next_pp()
                    for c in range(8):
                        R.op("pe", lambda e, c=c, ps=ps, c0=c0, cw=cw, tt=tt: e.matmul(
                            ps[:, 0:cw], lhsT=xn[:, c, tt * 128:(tt + 1) * 128],
                            rhs=wbf[:, c, vcol0 + c0:vcol0 + c0 + cw], start=(c == 0), stop=(c == 7)),
                             reads=[b_xn, b_w], writes=[bps])
                    g0 = c0 // vdim
                    ng = cw // vdim
                    R.op("act", lambda e, ps=ps, tt=tt, g0=g0, ng=ng, cw=cw, s=s: e.activation(
                        out=Vt[s][:, tt, g0:g0 + ng, 0:vdim],
                        in_=ps[:, 0:cw].rearrange("p (g v) -> p g v", v=vdim), func=AF.Copy),
                         reads=[bps], writes=[b_Vt[s]])
                    yield

        def oreg(o_ps, qc):
            if vd1 * NQC <= 512:
                return o_ps[:, qc * vd1:(qc + 1) * vd1]
            bk, i = qc // 2, qc % 2
            return o_ps[:, bk * 512 + i * vd1: bk * 512 + (i + 1) * vd1]

        def emit_qk(st):
            kb, m, qb, u, kt = st
            s = kb % 2
            kc, poff, qch, vg = maps[m]
            si = u * 4 + kt
            sps, bs = ps_S[si % 2][:], bS[si % 2]
            R.op("pe", lambda e: e.matmul(
                sps[:, 0:QB], lhsT=KT[s][poff:poff + 64, kc, kt * 128:(kt + 1) * 128],
                rhs=QT[poff:poff + 64, qch, qb * QB:(qb + 1) * QB], start=True, stop=True),
                 reads=[b_KT[s], b_QT], writes=[bs])
            pb, bpb = Pb[si % 3], b_Pb[si % 3]
            R.op("act", lambda e: e.activation(out=pb[:, 0:QB], in_=sps[:, 0:QB], func=AF.Exp, scale=0.125),
                 reads=[bs], writes=[bpb])

        def emit_pv(st):
            kb, m, qb, u, kt = st
            s = kb % 2
            kc, poff, qch, vg = maps[m]
            si = u * 4 + kt
            pb, bpb = Pb[si % 3], b_Pb[si % 3]
            o_ps, b_o = ps_O[u % 2][:], bO[u % 2]
            for qc in range(NQC):
                R.op("pe", lambda e, qc=qc: e.matmul(
                    oreg(o_ps, qc), lhsT=pb[:, qc * 128:(qc + 1) * 128], rhs=Vt[s][:, kt, vg, 0:vd1],
                    start=(kt == 0 and (qc == 0 if vd1 * NQC <= 512 else qc % 2 == 0)), stop=(kt == 3),
                    skip_group_check=True), reads=[bpb, b_Vt[s]], writes=[b_o])
            if kt != 3:
                return
            qi0 = qb * NQC
            if vd1 * NQC <= 512:
                groups = [(0, NQC, o_ps[:, 0:NQC * vd1])]
            else:
                groups = [(bk * 2, 2, o_ps[:, bk * 512: bk * 512 + 2 * vd1]) for bk in range(NQC // 2)]
            for (q0, nq, src) in groups:
                dst = acc[:, m, qi0 + q0:qi0 + q0 + nq, :]
                srcv = src.rearrange("p (q v) -> p q v", v=vd1)
                if kb == 0:
                    R.op("dve", lambda e, dst=dst, srcv=srcv: e.tensor_copy(out=dst, in_=srcv),
                         reads=[b_o], writes=[b_acc])
                else:
                    R.op("dve", lambda e, dst=dst, srcv=srcv: e.tensor_tensor(out=dst, in0=dst, in1=srcv,
                                                                              op=ALU.add),
                         reads=[b_o, b_acc], writes=[b_acc])

        all_steps = []
        u = 0
        for kb in range(NKB):
            for m in range(nmap):
                for qb in range(NQB):
                    for kt in range(4):
                        all_steps.append((kb, m, qb, u, kt))
                    u += 1
        run(inproj(0))
        gen = None
        emit_qk(all_steps[0])
        for i, st in enumerate(all_steps):
            kb = st[0]
            if (i == 0 or all_steps[i - 1][0] != kb) and kb + 1 < NKB:
                gen = inproj(kb + 1)
            if i + 1 < len(all_steps):
                if all_steps[i + 1][0] != kb and gen is not None:
                    run(gen)
                    gen = None
                emit_qk(all_steps[i + 1])
            emit_pv(st)
            if gen is not None and i % 2 == 1:
                try:
                    next(gen)
                except StopIteration:
                    gen = None

        fin = A.alloc([nmap + 2 * nmap + 8], F32)
        b_fin = Buf(tag + "fin")
        if diff:
            o_t = A.alloc([4, 128], F32)
            o_u = A.alloc([4, 128], F32)
            b_ot = Buf(tag + "ot")
            b_ou = Buf(tag + "ou")
        for qi in range(NQI):
            rl = fin[:, 0:nmap]
            R.op("dve", lambda e, qi=qi: e.reciprocal(out=rl, in_=acc[:, :, qi, vdim]), reads=[b_acc], writes=[b_fin])
            if not diff:
                R.op("dve", lambda e, qi=qi: e.tensor_tensor(
                    out=Otok[:, qi, ocol0:ocol0 + nmap * vdim].rearrange("p (h v) -> p h v", v=vdim),
                    in0=acc[:, :, qi, 0:vdim], in1=rl.unsqueeze(2).to_broadcast([128, nmap, vdim]), op=ALU.mult),
                     reads=[b_acc, b_fin], writes=[b_Otok[qi]])
            else:
                rl3 = rl.rearrange("p (h c) -> p h c", c=2)
                rl1 = fin[:, nmap:nmap + 4]
                R.op("dve", lambda e: e.tensor_scalar(out=rl1, in0=rl3[:, :, 1], scalar1=lam, scalar2=None,
                                                      op0=ALU.mult), reads=[b_fin, b_small], writes=[b_fin])
                accv = acc.rearrange("p (h c) q v -> p h c q v", c=2)
                R.op("dve", lambda e, qi=qi: e.tensor_tensor(
                    out=o_t, in0=accv[:, :, 0, qi, 0:vdim],
                    in1=rl3[:, :, 0].unsqueeze(2).to_broadcast([128, 4, vdim]), op=ALU.mult),
                     reads=[b_acc, b_fin], writes=[b_ot])
                R.op("dve", lambda e, qi=qi: e.tensor_tensor(
                    out=o_u, in0=accv[:, :, 1, qi, 0:vdim],
                    in1=rl1.unsqueeze(2).to_broadcast([128, 4, vdim]), op=ALU.mult),
                     reads=[b_acc, b_fin], writes=[b_ou])
                R.op("dve", lambda e: e.tensor_tensor(out=o_t, in0=o_t, in1=o_u, op=ALU.subtract),
                     reads=[b_ot, b_ou], writes=[b_ot])
                R.op("dve", lambda e: e.tensor_tensor(out=o_u, in0=o_t, in1=o_t, op=ALU.mult),
                     reads=[b_ot], writes=[b_ou])
                ssq = fin[:, nmap + 4:nmap + 8]
                R.op("dve", lambda e: e.reduce_sum(out=ssq, in_=o_u, axis=AX.X), reads=[b_ou], writes=[b_fin])
                rsqrt_mean("dve", ssq, ssq, float(vdim), SUBLN_EPS, [b_fin], [b_fin])
                R.op("dve", lambda e: e.tensor_tensor(out=o_t, in0=o_t,
                                                      in1=ssq.unsqueeze(2).to_broadcast([128, 4, vdim]), op=ALU.mult),
                     reads=[b_ot, b_fin], writes=[b_ot])
                R.op("dve", lambda e, qi=qi: e.tensor_tensor(
                    out=Otok[:, qi, ocol0:ocol0 + 4 * vdim].rearrange("p (h v) -> p h v", v=vdim),
                    in0=o_t, in1=subg.unsqueeze(1).to_broadcast([128, 4, vdim]), op=ALU.mult),
                     reads=[b_ot, b_cst], writes=[b_Otok[qi]])

    import os
    stage = int(os.environ.get("KSTAGE", "3"))
    if stage < 3:
        for qi in range(NQI):
            R.op("pool", lambda e, qi=qi: e.memset(Otok[:, qi, :], 0.0), writes=[b_Otok[qi]])
    mapsA = [(0, (h // 4) * 64, h % 4, h // 4) for h in range(8)]
    if stage >= 2:
        attn_phase("A", d_wA, 512, 1, 64, 2, True, d_tabA, d_tabAo, permA, mapsA, 0, False)
    mapsB = [(hh, comp * 64, hh, hh) for hh in range(4) for comp in range(2)]
    if stage >= 3:
        attn_phase("B", d_wB, 512, 4, 128, 4, False, d_tabB, d_tabBo, permB, mapsB, 512, True)

    A.off = phase_mark
    R.barrier()
    xn2T = A.alloc([8, TQ], BF16)
    b_xn2T = Buf("xn2T")
    f1_mark = A.off
    gffn = A.alloc([D], F32)
    b_gffn = Buf("gffn")
    R.dma("sp", lambda e: e.dma_start(out=gffn, in_=d_gffn), b_gffn, writes=[b_gffn])
    wout = A.alloc([8, D], BF16)
    b_wout = Buf("wout")
    wst = A.alloc([512], F32)
    b_wst = Buf("wst2")
    for c in range(8):
        for c0 in range(0, D, 512):
            R.dma("sp", lambda e, c=c, c0=c0: e.dma_start(out=wst, in_=d_wout[:, c, c0:c0 + 512]), b_wst,
                  writes=[b_wst])
            R.op("pool", lambda e, c=c, c0=c0: e.tensor_copy(out=wout[:, c, c0:c0 + 512], in_=wst),
                 reads=[b_wst], writes=[b_wout])
    OT = A.alloc([8, 128], BF16)
    b_OT = Buf("OT")
    xo = A.alloc([D], F32)
    b_xo = Buf("xo")
    hbuf = A.alloc([D], F32)
    b_h = Buf("h")
    junk = A.alloc([D], F32)
    b_junk = Buf("junk")
    obuf = A.alloc([D], F32)
    b_ob = Buf("obuf")
    xn2 = A.alloc([D], BF16)
    b_xn2 = Buf("xn2")
    st = A.alloc([8], F32)
    b_st = Buf("st")
    b_hs = [Buf("hs%d" % i) for i in range(NQI)]

    def final_norm(src, b_src, junk_, b_junk_, st_, b_st_, dst, b_dst, qi):
        R.op("act", lambda e: e.activation(out=junk_, in_=src, func=AF.Square, accum_out=st_[:, 0:1]),
             reads=[b_src], writes=[b_junk_, b_st_])
        rsqrt_mean("dve", st_[:, 1:2], st_[:, 0:1], float(D), NORM_EPS, [b_st_], [b_st_])
        R.op("dve", lambda e: e.scalar_tensor_tensor(out=dst, in0=src, scalar=st_[:, 1:2], in1=gfin,
                                                     op0=ALU.mult, op1=ALU.mult),
             reads=[b_src, b_st_, b_cst], writes=[b_dst])
        R.dma("sp", lambda e: e.dma_start(out=d_out[qi * 128:(qi + 1) * 128, :], in_=dst), b_dst, reads=[b_dst])

    def f1_tile(qi):
        R.dma("sp", lambda e: e.dma_start(out=xo, in_=d_xo[qi * 128:(qi + 1) * 128, :]), b_xo, writes=[b_xo])
        tp, btp = next_pp()
        tpb = tp.bitcast(BF16)
        for c in range(8):
            R.op("pe", lambda e, c=c: e.transpose(tpb[:, c * 128:(c + 1) * 128],
                                                  Otok[:, qi, c * 128:(c + 1) * 128], ident),
                 reads=[b_Otok[qi], b_cbf], writes=[btp])
        R.op("act", lambda e: e.activation(out=OT, in_=tpb.rearrange("p (c q) -> p c q", q=128), func=AF.Copy),
             reads=[btp], writes=[b_OT])
        for hf in range(2):
            ps, bps = next_pp()
            for c in range(8):
                R.op("pe", lambda e, c=c, hf=hf, ps=ps: e.matmul(ps, lhsT=OT[:, c, :],
                                                                 rhs=wout[:, c, hf * 512:(hf + 1) * 512],
                                                                 start=(c == 0), stop=(c == 7)),
                     reads=[b_OT, b_wout], writes=[bps])
            R.op("dve", lambda e, hf=hf, ps=ps: e.tensor_tensor(out=hbuf[:, hf * 512:(hf + 1) * 512], in0=ps,
                                                                in1=xo[:, hf * 512:(hf + 1) * 512], op=ALU.add),
                 reads=[bps, b_xo], writes=[b_h])
        if not with_peer:
            final_norm(hbuf, b_h, junk, b_junk, st, b_st, obuf, b_ob, qi)
            return
        R.dma("sp", lambda e: e.dma_start(out=d_hs[qi * 128:(qi + 1) * 128, :], in_=hbuf), b_h,
              reads=[b_h], writes=[b_hs[qi]])
        R.op("act", lambda e: e.activation(out=junk, in_=hbuf, func=AF.Square, accum_out=st[:, 0:1]),
             reads=[b_h], writes=[b_junk, b_st])
        rsqrt_mean("dve", st[:, 1:2], st[:, 0:1], float(D), NORM_EPS, [b_st], [b_st])
        R.op("dve", lambda e: e.scalar_tensor_tensor(out=xn2, in0=hbuf, scalar=st[:, 1:2], in1=gffn,
                                                     op0=ALU.mult, op1=ALU.mult),
             reads=[b_h, b_st, b_gffn], writes=[b_xn2])
        tp2, btp2 = next_pp()
        tpb2 = tp2.bitcast(BF16)
        for c in range(8):
            R.op("pe", lambda e, c=c: e.transpose(tpb2[:, c * 128:(c + 1) * 128], xn2[:, c * 128:(c + 1) * 128],
                                                  ident), reads=[b_xn2, b_cbf], writes=[btp2])
        R.op("act", lambda e: e.activation(out=xn2T[:, :, qi * 128:(qi + 1) * 128],
                                           in_=tpb2.rearrange("p (c q) -> p c q", q=128), func=AF.Copy),
             reads=[btp2], writes=[b_xn2T])

    for qi in range(NQI):
        f1_tile(qi)

    if with_peer:
        A.off = f1_mark
        A.n = ARENA_WORDS
        R.barrier()
        TB = min(4, NQI)
        NBK = NQI // TB
        NT = TB * 128
        NEG = -1.0e30
        skT = A.alloc([16, 128], BF16)
        b_skT = Buf("skT")
        sel = [A.alloc([16, 128], F32) for _ in range(TB)]
        b_sel = [Buf("sel%d" % i) for i in range(TB)]
        ssm = [A.alloc([64], F32) for _ in range(TB)]
        b_ssm = [Buf("ssm%d" % i) for i in range(TB)]
        Dg = [A.alloc([8, 128], BF16) for _ in range(TB)]
        b_Dg = [Buf("Dg%d" % i) for i in range(TB)]
        oacc = A.alloc([TB, D], F32)
        b_oacc = [Buf("oacc%d" % i) for i in range(TB)]
        st2 = A.alloc([8], F32)
        b_st2 = Buf("st2")
        union_mark = A.off
        qpT = A.alloc([16, NT], BF16)
        b_qpT = Buf("qpT")
        sc = A.alloc([16, 128], F32)
        b_sc = Buf("sc")
        cand = A.alloc([8, 256], F32)
        b_cand = Buf("cand")
        wqs = A.alloc([8, 128], F32)
        b_wqs = Buf("wqs")
        wqb = A.alloc([8, 128], BF16)
        b_wqb = Buf("wqb")
        vtop = A.alloc([16, 16], F32)
        b_vtop = Buf("vtop")
        top = A.alloc([8, 24], F32)
        b_top = Buf("top")
        mtmp = A.alloc([256], F32)
        b_mtmp = Buf("mtmp")
        d16 = A.alloc([8, 16], F32)
        b_d16 = Buf("d16")
        A.off = union_mark
        hb2 = A.alloc([D], F32)
        b_hb2 = Buf("hb2")
        jk2 = A.alloc([D], F32)
        b_jk2 = Buf("jk2")
        ob2 = A.alloc([D], F32)
        b_ob2 = Buf("ob2")
        A.off = union_mark
        ust = A.alloc([8, 512], F32)
        b_ust = Buf("ust")
        vst = A.alloc([4, D], F32)
        b_vst = Buf("vst")
        ubf = [A.alloc([8, 512], BF16) for _ in range(2)]
        b_ubf = [Buf("ubf%d" % i) for i in range(2)]
        vbf = [A.alloc([4, D], BF16) for _ in range(2)]
        b_vbf = [Buf("vbf%d" % i) for i in range(2)]
        Eb = [A.alloc([4, 512], F32) for _ in range(2)]
        b_Eb = [Buf("Eb%d" % i) for i in range(2)]
        Mb = A.alloc([8, 512], BF16)
        b_Mb = Buf("Mb")
        gaT = [A.alloc([4, NT], BF16) for _ in range(2)]
        b_gaT = [Buf("gaT%d" % i) for i in range(2)]
        HT = [A.alloc([4, 128], BF16) for _ in range(2)]
        b_HT = [Buf("HT%d" % i) for i in range(2)]
        ytmp = A.alloc([D], F32)
        b_ytmp = Buf("ytmp")

        skst = vst[:, 0:2, :].rearrange("p a (h n) -> p (a h) n", n=128)
        R.dma("sp", lambda e: e.dma_start(out=skst, in_=d_skT), b_vst, writes=[b_vst])
        R.op("dve", lambda e: e.tensor_copy(out=skT, in_=skst), reads=[b_vst], writes=[b_skT])
        uid = [0]

        def select_block(bk):
            t0 = bk * NT
            for hp in range(16):
                R.dma("sp", lambda e, hp=hp: e.dma_start(out=wqs, in_=d_wq[:, :, hp * 128:(hp + 1) * 128]), b_wqs,
                      writes=[b_wqs])
                R.op("pool", lambda e: e.tensor_copy(out=wqb, in_=wqs), reads=[b_wqs], writes=[b_wqb])
                ps, bps = next_pp()
                for c in range(8):
                    R.op("pe", lambda e, c=c, ps=ps: e.matmul(ps[:, 0:NT], lhsT=wqb[:, c, :],
                                                              rhs=xn2T[:, c, t0:t0 + NT], start=(c == 0),
                                                              stop=(c == 7)), reads=[b_wqb, b_xn2T], writes=[bps])
                R.op("act", lambda e, hp=hp, ps=ps: e.activation(out=qpT[:, hp, :], in_=ps[:, 0:NT], func=AF.Copy),
                     reads=[bps], writes=[b_qpT])
            for tt in range(TB):
                select_tile(tt)

        def select_tile(tt):
            sl, bsl, sm, bsm = sel[tt], b_sel[tt], ssm[tt], b_ssm[tt]
            for g in range(4):
                ps, bps = next_pp()
                for i in range(4):
                    hp = g * 4 + i
                    R.op("pe", lambda e, i=i, hp=hp, ps=ps: e.matmul(
                        ps[:, i * 128:(i + 1) * 128], lhsT=qpT[:, hp, tt * 128:(tt + 1) * 128], rhs=skT[:, hp, :],
                        start=True, stop=True, skip_group_check=True), reads=[b_qpT, b_skT], writes=[bps])
                R.op("act", lambda e, g=g, ps=ps: e.activation(
                    out=sc[:, g * 4:(g + 1) * 4, :], in_=ps.rearrange("p (a n) -> p a n", n=128), func=AF.Copy),
                     reads=[bps], writes=[b_sc])
            for hp in range(16):
                R.op("dve", lambda e, hp=hp: e.max(out=vtop[:, hp, 0:8], in_=sc[:, hp, :]), reads=[b_sc],
                     writes=[b_vtop])
                R.op("dve", lambda e, hp=hp: e.match_replace(out=mtmp[:, 0:128], in_to_replace=vtop[:, hp, 0:8],
                                                             in_values=sc[:, hp, :], imm_value=NEG),
                     reads=[b_sc, b_vtop], writes=[b_mtmp])
                R.op("dve", lambda e, hp=hp: e.max(out=vtop[:, hp, 8:16], in_=mtmp[:, 0:128]), reads=[b_mtmp],
                     writes=[b_vtop])
            vv = vtop.rearrange("p (h two) k -> p h two k", two=2)
            cand4 = cand.rearrange("p h (i j) -> p h i j", j=16)
            R.op("dve", lambda e: e.tensor_tensor(
                out=cand4, in0=vv[:, :, 0, :].unsqueeze(3).to_broadcast([128, 8, 16, 16]),
                in1=vv[:, :, 1, :].unsqueeze(2).to_broadcast([128, 8, 16, 16]), op=ALU.add),
                 reads=[b_vtop], writes=[b_cand])
            for h in range(8):
                R.op("dve", lambda e, h=h: e.max(out=top[:, h, 0:8], in_=cand[:, h, :]), reads=[b_cand],
                     writes=[b_top])
                R.op("dve", lambda e, h=h: e.match_replace(out=mtmp, in_to_replace=top[:, h, 0:8],
                                                           in_values=cand[:, h, :], imm_value=NEG),
                     reads=[b_cand, b_top], writes=[b_mtmp])
                R.op("dve", lambda e, h=h: e.max(out=top[:, h, 8:16], in_=mtmp), reads=[b_mtmp], writes=[b_top])
                R.op("dve", lambda e, h=h: e.match_replace(out=mtmp, in_to_replace=top[:, h, 8:16],
                                                           in_values=mtmp, imm_value=NEG),
                     reads=[b_mtmp, b_top], writes=[b_mtmp])
                R.op("dve", lambda e, h=h: e.max(out=top[:, h, 16:24], in_=mtmp), reads=[b_mtmp], writes=[b_top])
            mx = top[:, :, 0]
            R.op("dve", lambda e: e.tensor_tensor(out=d16, in0=top[:, :, 0:16],
                                                  in1=top[:, :, 0:1].to_broadcast([128, 8, 16]), op=ALU.subtract),
                 reads=[b_top], writes=[b_d16])
            R.op("act", lambda e: e.activation(out=d16, in_=d16, func=AF.Exp), reads=[b_d16], writes=[b_d16])
            R.op("dve", lambda e: e.reduce_sum(out=sm[:, 8:16], in_=d16, axis=AX.X), reads=[b_d16], writes=[bsm])
            R.op("dve", lambda e: e.reciprocal(out=sm[:, 8:16], in_=sm[:, 8:16]), reads=[bsm], writes=[bsm])
            R.op("dve", lambda e: e.tensor_tensor(out=sm[:, 16:24], in0=top[:, :, 15], in1=top[:, :, 16], op=ALU.add),
                 reads=[b_top], writes=[bsm])
            R.op("dve", lambda e: e.scalar_tensor_tensor(out=sm[:, 16:24], in0=sm[:, 16:24], scalar=0.5, in1=mx,
                                                         op0=ALU.mult, op1=ALU.subtract),
                 reads=[bsm, b_top], writes=[bsm])
            R.op("act", lambda e: e.activation(out=sm[:, 16:24], in_=sm[:, 16:24], func=AF.Exp), reads=[bsm],
                 writes=[bsm])
            R.op("dve", lambda e: e.tensor_tensor(out=sm[:, 0:8], in0=sm[:, 16:24], in1=sm[:, 8:16], op=ALU.mult),
                 reads=[bsm], writes=[bsm])
            R.op("dve", lambda e: e.reciprocal(out=sm[:, 24:32], in_=sm[:, 16:24]), reads=[bsm], writes=[bsm])
            R.op("dve", lambda e: e.tensor_tensor(out=sl, in0=sc, in1=vtop[:, :, 0:1].to_broadcast([128, 16, 128]),
                                                  op=ALU.subtract), reads=[b_sc, b_vtop], writes=[bsl])
            R.op("act", lambda e: e.activation(out=sl, in_=sl, func=AF.Exp), reads=[bsl], writes=[bsl])
            slv = sl.rearrange("p (h two) n -> p h two n", two=2)
            R.op("dve", lambda e: e.tensor_tensor(out=slv[:, :, 0, :], in0=slv[:, :, 0, :],
                                                  in1=sm[:, 24:32].unsqueeze(2).to_broadcast([128, 8, 128]),
                                                  op=ALU.mult), reads=[bsl, bsm], writes=[bsl])
            for h in range(8):
                R.op("dve", lambda e, h=h: e.tensor_scalar(out=Dg[tt][:, h, :], in0=ident, scalar1=sm[:, h:h + 1],
                                                           scalar2=None, op0=ALU.mult),
                     reads=[b_cbf, bsm], writes=[b_Dg[tt]])

        def load_chunk(j):
            k = j % 2
            R.dma("sp", lambda e: e.dma_start(out=ust, in_=d_uT[:, :, j * 512:(j + 1) * 512]), b_ust, writes=[b_ust])
            R.op("act", lambda e: e.activation(out=ubf[k], in_=ust, func=AF.Copy), reads=[b_ust], writes=[b_ubf[k]])
            R.dma("sp", lambda e: e.dma_start(
                out=vst, in_=d_ev[j * 512:(j + 1) * 512, :].rearrange("(a p) d -> p a d", p=128)), b_vst,
                  writes=[b_vst])
            R.op("act", lambda e: e.activation(out=vbf[k], in_=vst, func=AF.Copy), reads=[b_vst], writes=[b_vbf[k]])

        def chunk_gelu(bk, j):
            k = j % 2
            t0 = bk * NT
            for a in range(4):
                u = uid[0]
                uid[0] += 1
                psA, bA = ps_S[u % 2][:], bS[u % 2]
                for c in range(8):
                    R.op("pe", lambda e, c=c, a=a, psA=psA: e.matmul(
                        psA[:, 0:NT], lhsT=ubf[k][:, c, a * 128:(a + 1) * 128], rhs=xn2T[:, c, t0:t0 + NT],
                        start=(c == 0), stop=(c == 7)), reads=[b_ubf[k], b_xn2T], writes=[bA])
                R.op("act", lambda e, a=a, psA=psA: e.activation(out=gaT[k][:, a, :], in_=psA[:, 0:NT], func=AF.Gelu),
                     reads=[bA], writes=[b_gaT[k]])

        b_Mbh = [Buf("Mb%d" % i) for i in range(2)]

        def gates_E(tt, j):
            slv = sel[tt].rearrange("p (h two) n -> p h two n", two=2)
            R.op("dve", lambda e: e.tensor_tensor(
                out=Eb[0].rearrange("p h (a n) -> p h a n", n=128),
                in0=slv[:, 0:4, 0, 4 * j:4 * j + 4].unsqueeze(3).to_broadcast([128, 4, 4, 128]),
                in1=slv[:, 0:4, 1, :].unsqueeze(2).to_broadcast([128, 4, 4, 128]), op=ALU.mult),
                 reads=[b_sel[tt]], writes=[b_Eb[0]])
            E1v = Eb[1].rearrange("p h (a n) -> p h a n", n=128)
            for h in range(4):
                for a in range(4):
                    R.op("act", lambda e, h=h, a=a: e.activation(
                        out=E1v[:, h, a, :], in_=slv[:, 4 + h, 1, :], func=AF.Copy,
                        scale=slv[:, 4 + h, 0, 4 * j + a:4 * j + a + 1]), reads=[b_sel[tt]], writes=[b_Eb[1]])

        def gates_M(tt, j):
            for hh in range(2):
                R.op("dve", lambda e, hh=hh: e.scalar_tensor_tensor(
                    out=Mb[:, hh * 4:(hh + 1) * 4, :], in0=Eb[hh], scalar=1.0, in1=Eb[hh], op0=ALU.is_ge,
                    op1=ALU.mult), reads=[b_Eb[hh]], writes=[b_Mbh[hh]])

        def transposes(tt, j):
            gp, bgp = next_pp()
            for h in range(8):
                for a in range(4):
                    R.op("pe", lambda e, a=a, h=h: e.matmul(
                        gp[:, a * 128:(a + 1) * 128], lhsT=Mb[:, h, a * 128:(a + 1) * 128], rhs=Dg[tt][:, h, :],
                        start=(h == 0 and a == 0), stop=(h == 7), skip_group_check=True),
                         reads=[b_Mbh[h // 4], b_Dg[tt]], writes=[bgp])
            return gp, bgp

        def make_HT(tt, j, u, gp, bgp):
            k = j % 2
            htu, bhtu = HT[u % 2], b_HT[u % 2]
            R.op("dve", lambda e: e.tensor_tensor(out=htu, in0=gp.rearrange("p (a t) -> p a t", t=128),
                                                  in1=gaT[k][:, :, tt * 128:(tt + 1) * 128], op=ALU.mult),
                 reads=[bgp, b_gaT[k]], writes=[bhtu])

        def final(tt, j, u):
            k = j % 2
            htu, bhtu = HT[u % 2], b_HT[u % 2]
            psY, bY = ps_O[u % 2][:], bO[u % 2]
            for hf in range(2):
                for a in range(4):
                    R.op("pe", lambda e, a=a, hf=hf: e.matmul(
                        psY[:, hf * 512:(hf + 1) * 512], lhsT=htu[:, a, :], rhs=vbf[k][:, a, hf * 512:(hf + 1) * 512],
                        start=(a == 0), stop=(a == 3)), reads=[bhtu, b_vbf[k]], writes=[bY])
            if j == 0:
                R.op("act", lambda e: e.activation(out=oacc[:, tt, :], in_=psY, func=AF.Copy), reads=[bY],
                     writes=[b_oacc[tt]])
            else:
                R.op("act", lambda e: e.activation(out=ytmp, in_=psY, func=AF.Copy), reads=[bY], writes=[b_ytmp])
                R.op("pool", lambda e: e.tensor_tensor(out=oacc[:, tt, :], in0=oacc[:, tt, :], in1=ytmp, op=ALU.add),
                     reads=[b_ytmp, b_oacc[tt]], writes=[b_oacc[tt]])

        NCH = NE // 512
        for bk in range(NBK):
            R.barrier()
            select_block(bk)
            R.barrier()
            load_chunk(0)
            chunk_gelu(bk, 0)
            ulist = [(j, tt) for j in range(NCH) for tt in range(TB)]
            gates_E(ulist[0][1], ulist[0][0])
            gates_M(ulist[0][1], ulist[0][0])
            for i, (j, tt) in enumerate(ulist):
                u = uid[0]
                uid[0] += 1
                if tt == 0 and j + 1 < NCH:
                    load_chunk(j + 1)
                gp, bgp = transposes(tt, j)
                nxt = ulist[i + 1] if i + 1 < len(ulist) else None
                if nxt is not None:
                    gates_E(nxt[1], nxt[0])
                make_HT(tt, j, u, gp, bgp)
                if nxt is not None:
                    gates_M(nxt[1], nxt[0])
                final(tt, j, u)
                if tt == min(1, TB - 1) and j + 1 < NCH:
                    chunk_gelu(bk, j + 1)
            R.barrier()
            for tt in range(TB):
                qi = bk * TB + tt

                def epilogue(tt=tt, qi=qi):
                    R.dma("sp", lambda e: e.dma_start(out=hb2, in_=d_hs[qi * 128:(qi + 1) * 128, :]), b_hb2,
                          reads=[b_hs[qi]], writes=[b_hb2])
                    R.op("dve", lambda e: e.tensor_tensor(out=hb2, in0=hb2, in1=oacc[:, tt, :], op=ALU.add),
                         reads=[b_hb2, b_oacc[tt]], writes=[b_hb2])
                    final_norm(hb2, b_hb2, jk2, b_jk2, st2, b_st2, ob2, b_ob2, qi)
                epilogue()

    counts = R.finish(nc, stack)
    stack.close()
    print('[kernel] arena peak words', A.peak, 'of', ARENA_WORDS, 'sem incs', counts)
    return nc, counts, A.peak


def _chunk_rows(w):
    n = w.shape[1]
    return np.ascontiguousarray(w.reshape(8, 128, n).transpose(1, 0, 2))


def _tables(S):
    f32 = np.float32
    t = np.arange(S)
    row = (t // 64).astype(f32)
    col = (t % 64).astype(f32)
    pos = t.astype(f32)
    inv_ax = (f32(10000.0) ** (-np.arange(0, 32, 2, dtype=f32) / f32(32))).astype(f32)
    inv_p = (f32(500000.0) ** (-np.arange(0, 16, 2, dtype=f32) / f32(16))).astype(f32)
    ra = (row[:, None] * inv_ax[None, :]).astype(f32)
    ca = (col[:, None] * inv_ax[None, :]).astype(f32)
    pa = (pos[:, None] * inv_p[None, :]).astype(f32)
    cA = np.concatenate([np.cos(ra), np.cos(ra), np.cos(ca), np.cos(ca)], axis=1).T
    sA = np.concatenate([-np.sin(ra), np.sin(ra), -np.sin(ca), np.sin(ca)], axis=1).T
    cB = np.concatenate([np.cos(pa), np.cos(pa), np.ones((S, 48), f32)], axis=1).T
    sB = np.concatenate([-np.sin(pa), np.sin(pa), np.zeros((S, 48), f32)], axis=1).T
    tabA = np.stack([np.tile(cA, (2, 1)), np.tile(sA, (2, 1))], axis=1).astype(f32)
    tabB = np.stack([np.tile(cB, (2, 1)), np.tile(sB, (2, 1))], axis=1).astype(f32)
    return np.ascontiguousarray(tabA), np.ascontiguousarray(tabB)


def _consts():
    ident = np.eye(128, dtype=np.float32)
    ones = np.ones((128, 128), np.float32)
    blk = np.zeros((128, 128), np.float32)
    blk[:64, :64] = 1
    blk[64:, 64:] = 1

    def perm(partner):
        p = np.zeros((128, 128), np.float32)
        for m in range(128):
            p[(m // 64) * 64 + partner(m % 64), m] = 1
        return p

    def pa(d):
        return d + 16 if (d % 32) < 16 else d - 16

    def pb(d):
        if d < 8:
            return d + 8
        if d < 16:
            return d - 8
        return d

    return np.ascontiguousarray(np.stack([ident, ones, blk, perm(pa), perm(pb)], axis=1))


_CACHE = {}


def kernel(x, norm_attn_g, w_in, q_norm_g, k_norm_g, lambda_q1, lambda_k1, lambda_q2, lambda_k2, subln_g,
           w_out, norm_ffn_g, w_query, sub_keys, expert_u, expert_v, norm_final_g, _with_peer=True):
    f32 = np.float32
    x = np.asarray(x, f32)
    S = x.shape[1]
    TQ = S // NCORE
    x2 = x[0]
    xT = np.ascontiguousarray(x2.T.reshape(8, 128, S).transpose(1, 0, 2))
    w = np.asarray(w_in, f32)[0]
    qa, ka, va = w[:, 0:512], w[:, 512:640], w[:, 640:768]
    qb, kb, vb = w[:, 768:1280], w[:, 1280:1792], w[:, 1792:2304]
    qa_p = np.concatenate([np.concatenate([qa[:, j * 64:(j + 1) * 64], qa[:, (j + 4) * 64:(j + 5) * 64]], axis=1)
                           for j in range(4)], axis=1)
    wA = _chunk_rows(np.concatenate([qa_p, ka, va], axis=1))
    wB = _chunk_rows(np.concatenate([qb, kb, vb], axis=1))
    wout = _chunk_rows(np.asarray(w_out, f32)[0])
    tabA, tabB = _tables(S)
    vec = np.zeros((128, 18), f32)
    vec[:, 0:8] = np.asarray(norm_attn_g, f32)[0].reshape(8, 128).T
    vec[:, 8:16] = np.asarray(norm_ffn_g, f32)[0].reshape(8, 128).T
    vec[:, 16] = np.tile(np.asarray(q_norm_g, f32)[0], 2)
    vec[:, 17] = np.tile(np.asarray(k_norm_g, f32)[0], 2)
    lam = np.stack([np.asarray(a, f32)[0] for a in (lambda_q1, lambda_k1, lambda_q2, lambda_k2)], axis=0)
    lam = np.ascontiguousarray(np.broadcast_to(lam[None], (128, 4, 64)))
    sub = np.ascontiguousarray(np.broadcast_to(np.asarray(subln_g, f32)[0][None], (128, 128)))
    gfin = np.ascontiguousarray(np.broadcast_to(np.asarray(norm_final_g, f32)[None], (128, D)))
    gffn = np.ascontiguousarray(np.broadcast_to(np.asarray(norm_ffn_g, f32)[0][None], (128, D)))
    cst = _consts()
    key = (S, _with_peer)
    if key not in _CACHE:
        _CACHE[key] = build(S, _with_peer)
    nc, counts, peak = _CACHE[key]
    common = dict(xT=xT, wA=wA, wB=wB, wout=wout, cst=cst, tabA=tabA, tabB=tabB, vec=vec, lam=lam, subln=sub,
                  gfin=gfin, gffn=gffn)
    if _with_peer:
        common["wq"] = _chunk_rows(np.asarray(w_query, f32)[0])
        sk = np.asarray(sub_keys, f32)[0].reshape(16, 128, 128)
        common["skT"] = np.ascontiguousarray(sk.transpose(2, 0, 1))
        common["uT"] = _chunk_rows(np.ascontiguousarray(np.asarray(expert_u, f32)[0].T))
        common["ev"] = np.ascontiguousarray(np.asarray(expert_v, f32)[0])
    in_maps = []
    for c in range(NCORE):
        sl = slice(c * TQ, (c + 1) * TQ)
        m = dict(common)
        m["xTo"] = np.ascontiguousarray(xT[:, :, sl])
        m["xo"] = np.ascontiguousarray(x2[sl])
        m["tabAo"] = np.ascontiguousarray(tabA[:, :, sl])
        m["tabBo"] = np.ascontiguousarray(tabB[:, :, sl])
        in_maps.append(m)
    res = run_bass_kernel_spmd(nc, in_maps, core_ids=list(range(NCORE)))
    out = np.concatenate([np.asarray(r["out"], f32) for r in res.results], axis=0)
    return out.reshape(1, S, D)
```

```python
import math
from contextlib import ExitStack

import numpy as np
import concourse.bass as bass
import concourse.mybir as mybir
from concourse.bass_utils import run_bass_kernel_spmd

F32 = mybir.dt.float32
BF16 = mybir.dt.bfloat16
ALU = mybir.AluOpType
AF = mybir.ActivationFunctionType
AX = mybir.AxisListType

NCORE = 8
D = 1024
NORM_EPS = 1e-6
SUBLN_EPS = 1e-5
LAMBDA_INIT = 0.8 - 0.6 * math.exp(-0.3 * 0)
ENGS = ["pe", "act", "dve", "pool", "sp"]


class Buf:
    __slots__ = ("name", "w", "r", "dcnt", "sem", "uses_dma", "excl")

    def __init__(self, name, excl=False):
        self.name = name
        self.excl = excl
        self.w = None
        self.r = {}
        self.dcnt = 0
        self.sem = None
        self.uses_dma = False


class Op:
    __slots__ = ("eng", "fn", "deps", "tok", "dma", "waits", "inc")

    def __init__(self, eng, fn, deps, tok, dma):
        self.eng = eng
        self.fn = fn
        self.deps = deps
        self.tok = tok
        self.dma = dma
        self.waits = []
        self.inc = False


class Rec:
    def __init__(self):
        self.ops = []
        self.cnt = {e: 0 for e in ENGS}
        self.dma_bufs = []
        self.pending = {e: set() for e in ENGS}

    def _deps(self, eng, reads, writes):
        deps = set()
        for b in reads:
            if b.w is not None:
                if not (b.w[0] == "e" and b.w[1] == eng and eng == "pe"):
                    deps.add(b.w)
            if b.excl:
                for t in b.r.values():
                    if not (t[0] == "e" and t[1] == eng):
                        deps.add(t)
        for b in writes:
            if b.w is not None and not (b.w[0] == "e" and b.w[1] == eng):
                deps.add(b.w)
            for t in b.r.values():
                if not (t[0] == "e" and t[1] == eng):
                    deps.add(t)
        if self.pending[eng]:
            deps |= self.pending[eng]
            self.pending[eng] = set()
        return deps

    def op(self, eng, fn, reads=(), writes=()):
        deps = self._deps(eng, reads, writes)
        self.cnt[eng] += 1
        tok = ("e", eng, self.cnt[eng])
        self.ops.append(Op(eng, fn, deps, tok, None))
        for b in writes:
            b.w = tok
            b.r = {}
        for b in reads:
            b.r[eng] = tok
        return tok

    def dma(self, eng, fn, anchor, reads=(), writes=()):
        deps = self._deps(eng, reads, writes)
        if not anchor.uses_dma:
            anchor.uses_dma = True
            self.dma_bufs.append(anchor)
        anchor.dcnt += 16
        tok = ("d", anchor, anchor.dcnt)
        self.ops.append(Op(eng, fn, deps, tok, anchor))
        for b in writes:
            b.w = tok
            b.r = {}
        for b in reads:
            b.r[("d", anchor)] = tok
        return tok

    def barrier(self):
        toks = set()
        for e in ENGS:
            if e != "sp" and self.cnt[e] > 0:
                toks.add(("e", e, self.cnt[e]))
        for b in self.dma_bufs:
            toks.add(("d", b, b.dcnt))
        for e in ENGS:
            self.pending[e] |= {t for t in toks if not (t[0] == "e" and t[1] == e)}

    def finish(self, nc, stack):
        self.barrier()
        self.ops.append(Op("sp", None, self.pending["sp"], None, None))
        self.pending["sp"] = set()
        needed = set()
        waited = {e: {} for e in ENGS}
        for op in self.ops:
            w = waited[op.eng]
            for d in sorted(op.deps, key=lambda t: (t[0], str(t[1] if t[0] == "e" else t[1].name), t[2])):
                key = d[1]
                if w.get(key, 0) < d[2]:
                    w[key] = d[2]
            per = {}
            for d in op.deps:
                key = d[1]
                if key not in per or per[key][2] < d[2]:
                    per[key] = d
            op.waits = list(per.values())
        waited = {e: {} for e in ENGS}
        for op in self.ops:
            w = waited[op.eng]
            keep = []
            for d in op.waits:
                key = d[1]
                if w.get(key, 0) < d[2]:
                    w[key] = d[2]
                    keep.append(d)
                    if d[0] == "e":
                        needed.add((d[1], d[2]))
            op.waits = keep
        incval = {}
        c = {e: 0 for e in ENGS}
        for op in self.ops:
            if op.tok is not None and op.tok[0] == "e" and (op.tok[1], op.tok[2]) in needed:
                c[op.eng] += 1
                incval[(op.tok[1], op.tok[2])] = c[op.eng]
                op.inc = True
        esem = {e: stack.enter_context(nc.semaphore("es_" + e)) for e in ENGS if e != "sp"}
        for b in self.dma_bufs:
            b.sem = stack.enter_context(nc.semaphore("ds_" + b.name))
        per_eng = {e: [o for o in self.ops if o.eng == e] for e in ENGS}

        def emit(engobj, ename):
            for op in per_eng[ename]:
                for d in op.waits:
                    if d[0] == "e":
                        engobj.wait_ge(esem[d[1]], incval[(d[1], d[2])])
                    else:
                        engobj.wait_ge(d[1].sem, d[2])
                if op.fn is None:
                    continue
                ins = op.fn(engobj)
                if op.dma is not None:
                    ins.then_inc(op.dma.sem, 16)
                elif op.inc:
                    ins.then_inc(esem[ename], 1)

        with nc.Block() as block:
            @block.sync
            def _(e):
                emit(e, "sp")

            @block.tensor
            def _(e):
                emit(e, "pe")

            @block.scalar
            def _(e):
                emit(e, "act")

            @block.vector
            def _(e):
                emit(e, "dve")

            @block.gpsimd
            def _(e):
                emit(e, "pool")
        return {e: c[e] for e in ENGS}


class Arena:
    def __init__(self, ap, nwords):
        self.ap = ap
        self.n = nwords
        self.off = 0
        self.peak = 0

    def alloc(self, shape, dtype, top=False):
        n = int(np.prod(shape))
        words = n if dtype == F32 else (n + 1) // 2
        if top:
            self.n -= words
            a = self.ap[:, self.n:self.n + words]
        else:
            a = self.ap[:, self.off:self.off + words]
            self.off += words
        self.peak = max(self.peak, self.off)
        assert self.off <= self.n, ("arena overflow", self.off, self.n)
        if dtype != F32:
            a = a.bitcast(dtype)
        if len(shape) == 1:
            return a
        if len(shape) == 2:
            return a.rearrange("p (a b) -> p a b", b=shape[1])
        if len(shape) == 3:
            return a.rearrange("p (a b c) -> p a b c", b=shape[1], c=shape[2])
        if len(shape) == 4:
            return a.rearrange("p (a b c d) -> p a b c d", b=shape[1], c=shape[2], d=shape[3])
        raise ValueError(shape)


def build(S, with_peer=True):
    TQ = S // NCORE
    QB = min(512, TQ)
    NQB = TQ // QB
    NQC = QB // 128
    NQI = TQ // 128
    KB = 512
    NKB = S // KB
    NE = 16384
    nc = bass.Bass("TRN2", target_bir_lowering=False)
    R = Rec()

    def din(name, shape):
        return nc.dram_tensor(name, list(shape), F32, kind="ExternalInput").ap()

    d_xT = din("xT", [128, 8, S])
    d_xTo = din("xTo", [128, 8, TQ])
    d_xo = din("xo", [TQ, D])
    d_wA = din("wA", [128, 8, 768])
    d_wB = din("wB", [128, 8, 1536])
    d_wout = din("wout", [128, 8, D])
    d_cst = din("cst", [128, 5, 128])
    d_tabA = din("tabA", [128, 2, S])
    d_tabB = din("tabB", [128, 2, S])
    d_tabAo = din("tabAo", [128, 2, TQ])
    d_tabBo = din("tabBo", [128, 2, TQ])
    d_vec = din("vec", [128, 8 + 8 + 2])
    d_lam = din("lam", [128, 4, 64])
    d_sub = din("subln", [128, 128])
    d_gfin = din("gfin", [128, D])
    d_gffn = din("gffn", [128, D])
    if with_peer:
        d_wq = din("wq", [128, 8, 2048])
        d_skT = din("skT", [128, 16, 128])
        d_uT = din("uT", [128, 8, NE])
        d_ev = din("ev", [NE, D])
        d_hs = nc.dram_tensor("hscr", [TQ, D], F32, kind="Internal").ap()
    d_out = nc.dram_tensor("out", [TQ, D], F32, kind="ExternalOutput").ap()

    stack = ExitStack()
    ARENA_WORDS = 53200
    arena_t = stack.enter_context(nc.sbuf_tensor("arena", [128, ARENA_WORDS], F32))
    A = Arena(arena_t[:], ARENA_WORDS)
    ps_S = [stack.enter_context(nc.psum_tensor("psS%d" % i, [128, 512], F32)) for i in range(2)]
    ps_O = [stack.enter_context(nc.psum_tensor("psO%d" % i, [128, 1024], F32)) for i in range(2)]
    ps_P = [stack.enter_context(nc.psum_tensor("psP%d" % i, [128, 512], F32)) for i in range(2)]
    bS = [Buf("psS%d" % i, True) for i in range(2)]
    bO = [Buf("psO%d" % i, True) for i in range(2)]
    bP = [Buf("psP%d" % i, True) for i in range(2)]
    pp_i = [0]

    def next_pp():
        i = pp_i[0] % 2
        pp_i[0] += 1
        return ps_P[i][:], bP[i]

    cst_f = A.alloc([5, 128], F32)
    cst = A.alloc([5, 128], BF16)
    vec = A.alloc([18], F32)
    lam4 = A.alloc([4, 64], F32)
    subg = A.alloc([128], F32)
    gfin = A.alloc([D], F32)
    small = A.alloc([64], F32)
    b_cst = Buf("cst")
    b_small = Buf("small")
    ident = cst[:, 0, :]
    ones_bf = cst[:, 1, :]
    onesblk = cst[:, 2, :]
    permA = cst[:, 3, :]
    permB = cst[:, 4, :]
    Otok = A.alloc([NQI, D], BF16, top=True)
    b_Otok = [Buf("Otok%d" % i) for i in range(NQI)]

    R.dma("sp", lambda e: e.dma_start(out=cst_f, in_=d_cst), b_cst, writes=[b_cst])
    R.dma("sp", lambda e: e.dma_start(out=vec, in_=d_vec), b_cst, writes=[b_cst])
    R.dma("sp", lambda e: e.dma_start(out=lam4, in_=d_lam), b_cst, writes=[b_cst])
    R.dma("sp", lambda e: e.dma_start(out=subg, in_=d_sub), b_cst, writes=[b_cst])
    R.dma("sp", lambda e: e.dma_start(out=gfin, in_=d_gfin), b_cst, writes=[b_cst])
    b_cbf = Buf("cbf")
    R.op("dve", lambda e: e.tensor_copy(out=cst, in_=cst_f), reads=[b_cst], writes=[b_cbf])
    lt = A.alloc([2, 64], F32)
    R.op("dve", lambda e: e.tensor_tensor(out=lt[:, 0, :], in0=lam4[:, 0, :], in1=lam4[:, 1, :], op=ALU.mult),
         reads=[b_cst], writes=[b_small])
    R.op("dve", lambda e: e.tensor_tensor(out=lt[:, 1, :], in0=lam4[:, 2, :], in1=lam4[:, 3, :], op=ALU.mult),
         reads=[b_cst, b_small], writes=[b_small])
    R.op("dve", lambda e: e.reduce_sum(out=small[:, 2:4], in_=lt, axis=AX.X), reads=[b_small], writes=[b_small])
    R.op("act", lambda e: e.activation(out=small[:, 4:6], in_=small[:, 2:4], func=AF.Exp),
         reads=[b_small], writes=[b_small])
    R.op("dve", lambda e: e.tensor_tensor(out=small[:, 0:1], in0=small[:, 4:5], in1=small[:, 5:6], op=ALU.subtract),
         reads=[b_small], writes=[b_small])
    R.op("dve", lambda e: e.tensor_scalar(out=small[:, 0:1], in0=small[:, 0:1], scalar1=float(LAMBDA_INIT),
                                          scalar2=None, op0=ALU.add), reads=[b_small], writes=[b_small])
    R.op("dve", lambda e: e.tensor_scalar(out=subg, in0=subg, scalar1=float(1.0 - LAMBDA_INIT), scalar2=None,
                                          op0=ALU.mult), reads=[b_cst], writes=[b_cst])
    lam = small[:, 0:1]

    def rsqrt_mean(eng, out, in_, n, eps, reads, writes):
        R.op(eng, lambda e: e.tensor_scalar(out=out, in0=in_, scalar1=1.0 / n, scalar2=float(eps),
                                            op0=ALU.mult, op1=ALU.add), reads=reads, writes=writes)
        R.op("act", lambda e: e.activation(out=out, in_=out, func=AF.Ln), reads=writes, writes=writes)
        R.op("act", lambda e: e.activation(out=out, in_=out, func=AF.Exp, scale=-0.5), reads=writes, writes=writes)

    phase_mark = A.off

    def attn_phase(tag, d_w, ncol_q, nkc, vdim, ngroups, headnorm, d_tab, d_tabo, perm, maps, ocol0, diff):
        A.off = phase_mark
        R.barrier()
        ncol = ncol_q + nkc * 128 + ngroups * vdim
        kcol0 = ncol_q
        vcol0 = ncol_q + nkc * 128
        nqch = ncol_q // 128
        vd1 = vdim + 1
        nmap = len(maps)
        wbf = A.alloc([8, ncol], BF16)
        b_w = Buf(tag + "w")
        QT = A.alloc([nqch, TQ], BF16)
        b_QT = Buf(tag + "QT")
        acc = A.alloc([nmap, NQI, vd1], F32)
        b_acc = Buf(tag + "acc")
        xTb = A.alloc([8, 256], F32)
        b_xT = Buf(tag + "xT")
        sq = A.alloc([8, 256], BF16)
        b_sq = Buf(tag + "sq")
        rstd = A.alloc([256], F32)
        b_rstd = Buf(tag + "rstd")
        xn = A.alloc([8, 512], BF16)
        b_xn = Buf(tag + "xn")
        tab = A.alloc([2, 512], F32)
        b_tab = Buf(tag + "tab")
        KT = [A.alloc([nkc, 512], BF16) for _ in range(2)]
        b_KT = [Buf(tag + "KT%d" % i) for i in range(2)]
        Vt_flat = [A.alloc([4 * ngroups * (vdim + 2)], BF16) for _ in range(2)]
        Vt = [v.rearrange("p (t g v) -> p t g v", g=ngroups, v=vdim + 2) for v in Vt_flat]
        b_Vt = [Buf(tag + "Vt%d" % i) for i in range(2)]
        Pb = [A.alloc([512], BF16) for _ in range(3)]
        b_Pb = [Buf(tag + "P%d" % i) for i in range(3)]
        t_sq = A.alloc([512], BF16)
        t_rs = A.alloc([512], F32)
        t_n = A.alloc([512], BF16)
        t_1 = A.alloc([512], F32)
        t_2 = A.alloc([512], F32)
        b_tsq, b_trs, b_tn, b_t1, b_t2 = (Buf(tag + n) for n in ("tsq", "trs", "tn", "t1", "t2"))
        wst, b_wst = t_2, b_t2

        for c in range(8):
            for c0 in range(0, ncol, 512):
                cw = min(512, ncol - c0)
                R.dma("sp", lambda e, c=c, c0=c0, cw=cw: e.dma_start(out=wst[:, 0:cw], in_=d_w[:, c, c0:c0 + cw]),
                      b_wst, writes=[b_wst])
                R.op("dve", lambda e, c=c, c0=c0, cw=cw: e.tensor_scalar(
                    out=wbf[:, c, c0:c0 + cw], in0=wst[:, 0:cw], scalar1=vec[:, c:c + 1], scalar2=None,
                    op0=ALU.mult), reads=[b_wst, b_cst], writes=[b_w])
        for i in range(2):
            R.op("dve", lambda e, i=i: e.memset(Vt_flat[i], 1.0), writes=[b_Vt[i]])

        def norm_unit(src, dst):
            R.dma("sp", lambda e: e.dma_start(out=xTb, in_=src), b_xT, writes=[b_xT])
            R.op("act", lambda e: e.activation(out=sq, in_=xTb, func=AF.Square), reads=[b_xT], writes=[b_sq])
            yield
            ps, bps = next_pp()
            for c in range(8):
                R.op("pe", lambda e, c=c: e.matmul(ps[:, 0:256], lhsT=ones_bf, rhs=sq[:, c, :], start=(c == 0),
                                                   stop=(c == 7)), reads=[b_sq, b_cbf], writes=[bps])
            rsqrt_mean("dve", rstd, ps[:, 0:256], float(D), NORM_EPS, [bps], [b_rstd])
            R.op("dve", lambda e: e.tensor_tensor(out=dst, in0=xTb,
                                                  in1=rstd.unsqueeze(1).to_broadcast([128, 8, 256]), op=ALU.mult),
                 reads=[b_xT, b_rstd], writes=[b_xn])
            yield

        def run(gen):
            for _ in gen:
                pass

        def load_tab(src):
            R.dma("sp", lambda e: e.dma_start(out=tab, in_=src), b_tab, writes=[b_tab])

        def proj_post(wcol, n, gcol, dst, b_dst):
            ps, bps = next_pp()
            for c in range(8):
                R.op("pe", lambda e, c=c: e.matmul(ps[:, 0:n], lhsT=wbf[:, c, wcol:wcol + 128], rhs=xn[:, c, 0:n],
                                                   start=(c == 0), stop=(c == 7)), reads=[b_w, b_xn], writes=[bps])
            if headnorm:
                R.op("act", lambda e: e.activation(out=t_sq[:, 0:n], in_=ps[:, 0:n], func=AF.Square),
                     reads=[bps], writes=[b_tsq])
                yield
                ps2, bps2 = next_pp()
                R.op("pe", lambda e: e.matmul(ps2[:, 0:n], lhsT=onesblk, rhs=t_sq[:, 0:n], start=True, stop=True),
                     reads=[b_tsq, b_cbf], writes=[bps2])
                rsqrt_mean("dve", t_rs[:, 0:n], ps2[:, 0:n], 64.0, NORM_EPS, [bps2], [b_trs])
                R.op("dve", lambda e: e.scalar_tensor_tensor(out=t_1[:, 0:n], in0=ps[:, 0:n],
                                                             scalar=vec[:, gcol:gcol + 1], in1=t_rs[:, 0:n],
                                                             op0=ALU.mult, op1=ALU.mult),
                     reads=[bps, b_trs, b_cst], writes=[b_t1])
                R.op("pool", lambda e: e.tensor_copy(out=t_n[:, 0:n], in_=t_1[:, 0:n]), reads=[b_t1], writes=[b_tn])
                yield
                ps3, bps3 = bps2 and (ps2, bps2)
                R.op("pe", lambda e: e.matmul(ps3[:, 0:n], lhsT=perm, rhs=t_n[:, 0:n], start=True, stop=True),
                     reads=[b_tn, b_cbf], writes=[bps3])
                R.op("pool", lambda e: e.tensor_tensor(out=t_1[:, 0:n], in0=t_1[:, 0:n], in1=tab[:, 0, 0:n],
                                                       op=ALU.mult), reads=[b_t1, b_tab], writes=[b_t1])
            else:
                R.op("act", lambda e: e.activation(out=t_n[:, 0:n], in_=ps[:, 0:n], func=AF.Copy),
                     reads=[bps], writes=[b_tn])
                yield
                ps3, bps3 = next_pp()
                R.op("pe", lambda e: e.matmul(ps3[:, 0:n], lhsT=perm, rhs=t_n[:, 0:n], start=True, stop=True),
                     reads=[b_tn, b_cbf], writes=[bps3])
                R.op("dve", lambda e: e.tensor_tensor(out=t_1[:, 0:n], in0=ps[:, 0:n], in1=tab[:, 0, 0:n],
                                                      op=ALU.mult), reads=[bps, b_tab], writes=[b_t1])
            R.op("dve", lambda e: e.tensor_tensor(out=t_2[:, 0:n], in0=ps3[:, 0:n], in1=tab[:, 1, 0:n], op=ALU.mult),
                 reads=[bps3, b_tab], writes=[b_t2])
            R.op("pool", lambda e: e.tensor_tensor(out=dst, in0=t_1[:, 0:n], in1=t_2[:, 0:n], op=ALU.add),
                 reads=[b_t1, b_t2], writes=[b_dst])
            yield

        for qb in range(NQB):
            for hf in range(QB // 256 if QB >= 256 else 1):
                w_ = min(256, QB)
                t0 = qb * QB + hf * w_
                if w_ == 256:
                    run(norm_unit(d_xTo[:, :, t0:t0 + 256], xn[:, :, hf * 256:(hf + 1) * 256]))
                else:
                    R.dma("sp", lambda e, t0=t0: e.dma_start(out=xTb[:, :, 0:128], in_=d_xTo[:, :, t0:t0 + 128]),
                          b_xT, writes=[b_xT])
                    R.op("act", lambda e: e.activation(out=sq[:, :, 0:128], in_=xTb[:, :, 0:128], func=AF.Square),
                         reads=[b_xT], writes=[b_sq])
                    ps, bps = next_pp()
                    for c in range(8):
                        R.op("pe", lambda e, c=c, ps=ps: e.matmul(ps[:, 0:128], lhsT=ones_bf, rhs=sq[:, c, 0:128],
                                                                  start=(c == 0), stop=(c == 7)),
                             reads=[b_sq, b_cbf], writes=[bps])
                    rsqrt_mean("dve", rstd[:, 0:128], ps[:, 0:128], float(D), NORM_EPS, [bps], [b_rstd])
                    R.op("dve", lambda e: e.tensor_tensor(
                        out=xn[:, :, 0:128], in0=xTb[:, :, 0:128],
                        in1=rstd[:, 0:128].unsqueeze(1).to_broadcast([128, 8, 128]), op=ALU.mult),
                         reads=[b_xT, b_rstd], writes=[b_xn])
            load_tab(d_tabo[:, :, qb * QB:(qb + 1) * QB]) if QB == 512 else R.dma(
                "sp", lambda e, qb=qb: e.dma_start(out=tab[:, :, 0:QB], in_=d_tabo[:, :, qb * QB:(qb + 1) * QB]),
                b_tab, writes=[b_tab])
            for j in range(nqch):
                run(proj_post(j * 128, QB, 16, QT[:, j, qb * QB:(qb + 1) * QB], b_QT))

        def inproj(kb):
            s = kb % 2
            for hf in range(2):
                t0 = kb * KB + hf * 256
                yield from norm_unit(d_xT[:, :, t0:t0 + 256], xn[:, :, hf * 256:(hf + 1) * 256])
            load_tab(d_tab[:, :, kb * KB:(kb + 1) * KB])
            for kc in range(nkc):
                yield from proj_post(kcol0 + kc * 128, 512, 17, KT[s][:, kc, :], b_KT[s])
            for tt in range(4):
                vw = ngroups * vdim
                for c0 in range(0, vw, 512):
                    cw = min(512, vw - c0)
                    ps, bps = next_pp()
                    for c in range(8):
                        R.op("pe", lambda e, c=c, ps=ps, c0=c0, cw=cw, tt=tt: e.matmul(
                            ps[:, 0:cw], lhsT=xn[:, c, tt * 128:(tt + 1) * 128],
                            rhs=wbf[:, c, vcol0 + c0:vcol0 + c0 + cw], start=(c == 0), stop=(c == 7)),
                             reads=[b_xn, b_w], writes=[bps])
                    g0 = c0 // vdim
                    ng = cw // vdim
                    R.op("act", lambda e, ps=ps, tt=tt, g0=g0, ng=ng, cw=cw, s=s: e.activation(
                        out=Vt[s][:, tt, g0:g0 + ng, 0:vdim],
                        in_=ps[:, 0:cw].rearrange("p (g v) -> p g v", v=vdim), func=AF.Copy),
                         reads=[bps], writes=[b_Vt[s]])
                    yield

        def oreg(o_ps, qc):
            if vd1 * NQC <= 512:
                return o_ps[:, qc * vd1:(qc + 1) * vd1]
            bk, i = qc // 2, qc % 2
            return o_ps[:, bk * 512 + i * vd1: bk * 512 + (i + 1) * vd1]

        def emit_qk(st):
            kb, m, qb, u, kt = st
            s = kb % 2
            kc, poff, qch, vg = maps[m]
            si = u * 4 + kt
            sps, bs = ps_S[si % 2][:], bS[si % 2]
            R.op("pe", lambda e: e.matmul(
                sps[:, 0:QB], lhsT=KT[s][poff:poff + 64, kc, kt * 128:(kt + 1) * 128],
                rhs=QT[poff:poff + 64, qch, qb * QB:(qb + 1) * QB], start=True, stop=True),
                 reads=[b_KT[s], b_QT], writes=[bs])
            pb, bpb = Pb[si % 3], b_Pb[si % 3]
            R.op("act", lambda e: e.activation(out=pb[:, 0:QB], in_=sps[:, 0:QB], func=AF.Exp, scale=0.125),
                 reads=[bs], writes=[bpb])

        def emit_pv(st):
            kb, m, qb, u, kt = st
            s = kb % 2
            kc, poff, qch, vg = maps[m]
            si = u * 4 + kt
            pb, bpb = Pb[si % 3], b_Pb[si % 3]
            o_ps, b_o = ps_O[u % 2][:], bO[u % 2]
            for qc in range(NQC):
                R.op("pe", lambda e, qc=qc: e.matmul(
                    oreg(o_ps, qc), lhsT=pb[:, qc * 128:(qc + 1) * 128], rhs=Vt[s][:, kt, vg, 0:vd1],
                    start=(kt == 0 and (qc == 0 if vd1 * NQC <= 512 else qc % 2 == 0)), stop=(kt == 3),
                    skip_group_check=True), reads=[bpb, b_Vt[s]], writes=[b_o])
            if kt != 3:
                return
            qi0 = qb * NQC
            if vd1 * NQC <= 512:
                groups = [(0, NQC, o_ps[:, 0:NQC * vd1])]
            else:
                groups = [(bk * 2, 2, o_ps[:, bk * 512: bk * 512 + 2 * vd1]) for bk in range(NQC // 2)]
            for (q0, nq, src) in groups:
                dst = acc[:, m, qi0 + q0:qi0 + q0 + nq, :]
                srcv = src.rearrange("p (q v) -> p q v", v=vd1)
                if kb == 0:
                    R.op("dve", lambda e, dst=dst, srcv=srcv: e.tensor_copy(out=dst, in_=srcv),
                         reads=[b_o], writes=[b_acc])
                else:
                    R.op("dve", lambda e, dst=dst, srcv=srcv: e.tensor_tensor(out=dst, in0=dst, in1=srcv,
                                                                              op=ALU.add),
                         reads=[b_o, b_acc], writes=[b_acc])

        all_steps = []
        u = 0
        for kb in range(NKB):
            for m in range(nmap):
                for qb in range(NQB):
                    for kt in range(4):
                        all_steps.append((kb, m, qb, u, kt))
                    u += 1
        run(inproj(0))
        gen = None
        emit_qk(all_steps[0])
        for i, st in enumerate(all_steps):
            kb = st[0]
            if (i == 0 or all_steps[i - 1][0] != kb) and kb + 1 < NKB:
                gen = inproj(kb + 1)
            if i + 1 < len(all_steps):
                if all_steps[i + 1][0] != kb and gen is not None:
                    run(gen)
                    gen = None
                emit_qk(all_steps[i + 1])
            emit_pv(st)
            if gen is not None and i % 2 == 1:
                try:
                    next(gen)
                except StopIteration:
                    gen = None

        fin = A.alloc([nmap + 2 * nmap + 8], F32)
        b_fin = Buf(tag + "fin")
        if diff:
            o_t = A.alloc([4, 128], F32)
            o_u = A.alloc([4, 128], F32)
            b_ot = Buf(tag + "ot")
            b_ou = Buf(tag + "ou")
        for qi in range(NQI):
            rl = fin[:, 0:nmap]
            R.op("dve", lambda e, qi=qi: e.reciprocal(out=rl, in_=acc[:, :, qi, vdim]), reads=[b_acc], writes=[b_fin])
            if not diff:
                R.op("dve", lambda e, qi=qi: e.tensor_tensor(
                    out=Otok[:, qi, ocol0:ocol0 + nmap * vdim].rearrange("p (h v) -> p h v", v=vdim),
                    in0=acc[:, :, qi, 0:vdim], in1=rl.unsqueeze(2).to_broadcast([128, nmap, vdim]), op=ALU.mult),
                     reads=[b_acc, b_fin], writes=[b_Otok[qi]])
            else:
                rl3 = rl.rearrange("p (h c) -> p h c", c=2)
                rl1 = fin[:, nmap:nmap + 4]
                R.op("dve", lambda e: e.tensor_scalar(out=rl1, in0=rl3[:, :, 1], scalar1=lam, scalar2=None,
                                                      op0=ALU.mult), reads=[b_fin, b_small], writes=[b_fin])
                accv = acc.rearrange("p (h c) q v -> p h c q v", c=2)
                R.op("dve", lambda e, qi=qi: e.tensor_tensor(
                    out=o_t, in0=accv[:, :, 0, qi, 0:vdim],
                    in1=rl3[:, :, 0].unsqueeze(2).to_broadcast([128, 4, vdim]), op=ALU.mult),
                     reads=[b_acc, b_fin], writes=[b_ot])
                R.op("dve", lambda e, qi=qi: e.tensor_tensor(
                    out=o_u, in0=accv[:, :, 1, qi, 0:vdim],
                    in1=rl1.unsqueeze(2).to_broadcast([128, 4, vdim]), op=ALU.mult),
                     reads=[b_acc, b_fin], writes=[b_ou])
                R.op("dve", lambda e: e.tensor_tensor(out=o_t, in0=o_t, in1=o_u, op=ALU.subtract),
                     reads=[b_ot, b_ou], writes=[b_ot])
                R.op("dve", lambda e: e.tensor_tensor(out=o_u, in0=o_t, in1=o_t, op=ALU.mult),
                     reads=[b_ot], writes=[b_ou])
                ssq = fin[:, nmap + 4:nmap + 8]
                R.op("dve", lambda e: e.reduce_sum(out=ssq, in_=o_u, axis=AX.X), reads=[b_ou], writes=[b_fin])
                rsqrt_mean("dve", ssq, ssq, float(vdim), SUBLN_EPS, [b_fin], [b_fin])
                R.op("dve", lambda e: e.tensor_tensor(out=o_t, in0=o_t,
                                                      in1=ssq.unsqueeze(2).to_broadcast([128, 4, vdim]), op=ALU.mult),
                     reads=[b_ot, b_fin], writes=[b_ot])
                R.op("dve", lambda e, qi=qi: e.tensor_tensor(
                    out=Otok[:, qi, ocol0:ocol0 + 4 * vdim].rearrange("p (h v) -> p h v", v=vdim),
                    in0=o_t, in1=subg.unsqueeze(1).to_broadcast([128, 4, vdim]), op=ALU.mult),
                     reads=[b_ot, b_cst], writes=[b_Otok[qi]])

    import os
    stage = int(os.environ.get("KSTAGE", "3"))
    if stage < 3:
        for qi in range(NQI):
            R.op("pool", lambda e, qi=qi: e.memset(Otok[:, qi, :], 0.0), writes=[b_Otok[qi]])
    mapsA = [(0, (h // 4) * 64, h % 4, h // 4) for h in range(8)]
    if stage >= 2:
        attn_phase("A", d_wA, 512, 1, 64, 2, True, d_tabA, d_tabAo, permA, mapsA, 0, False)
    mapsB = [(hh, comp * 64, hh, hh) for hh in range(4) for comp in range(2)]
    if stage >= 3:
        attn_phase("B", d_wB, 512, 4, 128, 4, False, d_tabB, d_tabBo, permB, mapsB, 512, True)

    A.off = phase_mark
    R.barrier()
    xn2T = A.alloc([8, TQ], BF16)
    b_xn2T = Buf("xn2T")
    f1_mark = A.off
    gffn = A.alloc([D], F32)
    b_gffn = Buf("gffn")
    R.dma("sp", lambda e: e.dma_start(out=gffn, in_=d_gffn), b_gffn, writes=[b_gffn])
    wout = A.alloc([8, D], BF16)
    b_wout = Buf("wout")
    wst = A.alloc([512], F32)
    b_wst = Buf("wst2")
    for c in range(8):
        for c0 in range(0, D, 512):
            R.dma("sp", lambda e, c=c, c0=c0: e.dma_start(out=wst, in_=d_wout[:, c, c0:c0 + 512]), b_wst,
                  writes=[b_wst])
            R.op("pool", lambda e, c=c, c0=c0: e.tensor_copy(out=wout[:, c, c0:c0 + 512], in_=wst),
                 reads=[b_wst], writes=[b_wout])
    OT = A.alloc([8, 128], BF16)
    b_OT = Buf("OT")
    xo = A.alloc([D], F32)
    b_xo = Buf("xo")
    hbuf = A.alloc([D], F32)
    b_h = Buf("h")
    junk = A.alloc([D], F32)
    b_junk = Buf("junk")
    obuf = A.alloc([D], F32)
    b_ob = Buf("obuf")
    xn2 = A.alloc([D], BF16)
    b_xn2 = Buf("xn2")
    st = A.alloc([8], F32)
    b_st = Buf("st")
    b_hs = [Buf("hs%d" % i) for i in range(NQI)]

    def final_norm(src, b_src, junk_, b_junk_, st_, b_st_, dst, b_dst, qi):
        R.op("act", lambda e: e.activation(out=junk_, in_=src, func=AF.Square, accum_out=st_[:, 0:1]),
             reads=[b_src], writes=[b_junk_, b_st_])
        rsqrt_mean("dve", st_[:, 1:2], st_[:, 0:1], float(D), NORM_EPS, [b_st_], [b_st_])
        R.op("dve", lambda e: e.scalar_tensor_tensor(out=dst, in0=src, scalar=st_[:, 1:2], in1=gfin,
                                                     op0=ALU.mult, op1=ALU.mult),
             reads=[b_src, b_st_, b_cst], writes=[b_dst])
        R.dma("sp", lambda e: e.dma_start(out=d_out[qi * 128:(qi + 1) * 128, :], in_=dst), b_dst, reads=[b_dst])

    def f1_tile(qi):
        R.dma("sp", lambda e: e.dma_start(out=xo, in_=d_xo[qi * 128:(qi + 1) * 128, :]), b_xo, writes=[b_xo])
        tp, btp = next_pp()
        tpb = tp.bitcast(BF16)
        for c in range(8):
            R.op("pe", lambda e, c=c: e.transpose(tpb[:, c * 128:(c + 1) * 128],
                                                  Otok[:, qi, c * 128:(c + 1) * 128], ident),
                 reads=[b_Otok[qi], b_cbf], writes=[btp])
        R.op("act", lambda e: e.activation(out=OT, in_=tpb.rearrange("p (c q) -> p c q", q=128), func=AF.Copy),
             reads=[btp], writes=[b_OT])
        for hf in range(2):
            ps, bps = next_pp()
            for c in range(8):
                R.op("pe", lambda e, c=c, hf=hf, ps=ps: e.matmul(ps, lhsT=OT[:, c, :],
                                                                 rhs=wout[:, c, hf * 512:(hf + 1) * 512],
                                                                 start=(c == 0), stop=(c == 7)),
                     reads=[b_OT, b_wout], writes=[bps])
            R.op("dve", lambda e, hf=hf, ps=ps: e.tensor_tensor(out=hbuf[:, hf * 512:(hf + 1) * 512], in0=ps,
                                                                in1=xo[:, hf * 512:(hf + 1) * 512], op=ALU.add),
                 reads=[bps, b_xo], writes=[b_h])
        if not with_peer:
            final_norm(hbuf, b_h, junk, b_junk, st, b_st, obuf, b_ob, qi)
            return
        R.dma("sp", lambda e: e.dma_start(out=d_hs[qi * 128:(qi + 1) * 128, :], in_=hbuf), b_h,
              reads=[b_h], writes=[b_hs[qi]])
        R.op("act", lambda e: e.activation(out=junk, in_=hbuf, func=AF.Square, accum_out=st[:, 0:1]),
             reads=[b_h], writes=[b_junk, b_st])
        rsqrt_mean("dve", st[:, 1:2], st[:, 0:1], float(D), NORM_EPS, [b_st], [b_st])
        R.op("dve", lambda e: e.scalar_tensor_tensor(out=xn2, in0=hbuf, scalar=st[:, 1:2], in1=gffn,
                                                     op0=ALU.mult, op1=ALU.mult),
             reads=[b_h, b_st, b_gffn], writes=[b_xn2])
        tp2, btp2 = next_pp()
        tpb2 = tp2.bitcast(BF16)
        for c in range(8):
            R.op("pe", lambda e, c=c: e.transpose(tpb2[:, c * 128:(c + 1) * 128], xn2[:, c * 128:(c + 1) * 128],
                                                  ident), reads=[b_xn2, b_cbf], writes=[btp2])
        R.op("act", lambda e: e.activation(out=xn2T[:, :, qi * 128:(qi + 1) * 128],
                                           in_=tpb2.rearrange("p (c q) -> p c q", q=128), func=AF.Copy),
             reads=[btp2], writes=[b_xn2T])

    for qi in range(NQI):
        f1_tile(qi)

    if with_peer:
        A.off = f1_mark
        A.n = ARENA_WORDS
        R.barrier()
        TB = min(4, NQI)
        NBK = NQI // TB
        NT = TB * 128
        NEG = -1.0e30
        skT = A.alloc([16, 128], BF16)
        b_skT = Buf("skT")
        sel = [A.alloc([16, 128], F32) for _ in range(TB)]
        b_sel = [Buf("sel%d" % i) for i in range(TB)]
        ssm = [A.alloc([64], F32) for _ in range(TB)]
        b_ssm = [Buf("ssm%d" % i) for i in range(TB)]
        Dg = [A.alloc([8, 128], BF16) for _ in range(TB)]
        b_Dg = [Buf("Dg%d" % i) for i in range(TB)]
        oacc = A.alloc([TB, D], F32)
        b_oacc = [Buf("oacc%d" % i) for i in range(TB)]
        st2 = A.alloc([8], F32)
        b_st2 = Buf("st2")
        union_mark = A.off
        qpT = A.alloc([16, NT], BF16)
        b_qpT = Buf("qpT")
        sc = A.alloc([16, 128], F32)
        b_sc = Buf("sc")
        cand = A.alloc([8, 256], F32)
        b_cand = Buf("cand")
        wqs = A.alloc([8, 128], F32)
        b_wqs = Buf("wqs")
        wqb = A.alloc([8, 128], BF16)
        b_wqb = Buf("wqb")
        vtop = A.alloc([16, 16], F32)
        b_vtop = Buf("vtop")
        top = A.alloc([8, 24], F32)
        b_top = Buf("top")
        mtmp = A.alloc([256], F32)
        b_mtmp = Buf("mtmp")
        d16 = A.alloc([8, 16], F32)
        b_d16 = Buf("d16")
        A.off = union_mark
        hb2 = A.alloc([D], F32)
        b_hb2 = Buf("hb2")
        jk2 = A.alloc([D], F32)
        b_jk2 = Buf("jk2")
        ob2 = A.alloc([D], F32)
        b_ob2 = Buf("ob2")
        A.off = union_mark
        ust = A.alloc([8, 512], F32)
        b_ust = Buf("ust")
        vst = A.alloc([4, D], F32)
        b_vst = Buf("vst")
        ubf = [A.alloc([8, 512], BF16) for _ in range(2)]
        b_ubf = [Buf("ubf%d" % i) for i in range(2)]
        vbf = [A.alloc([4, D], BF16) for _ in range(2)]
        b_vbf = [Buf("vbf%d" % i) for i in range(2)]
        Eb = [A.alloc([4, 512], F32) for _ in range(2)]
        b_Eb = [Buf("Eb%d" % i) for i in range(2)]
        Mb = A.alloc([8, 512], BF16)
        b_Mb = Buf("Mb")
        gaT = [A.alloc([4, NT], BF16) for _ in range(2)]
        b_gaT = [Buf("gaT%d" % i) for i in range(2)]
        HT = [A.alloc([4, 128], BF16) for _ in range(2)]
        b_HT = [Buf("HT%d" % i) for i in range(2)]
        ytmp = A.alloc([D], F32)
        b_ytmp = Buf("ytmp")

        skst = vst[:, 0:2, :].rearrange("p a (h n) -> p (a h) n", n=128)
        R.dma("sp", lambda e: e.dma_start(out=skst, in_=d_skT), b_vst, writes=[b_vst])
        R.op("dve", lambda e: e.tensor_copy(out=skT, in_=skst), reads=[b_vst], writes=[b_skT])
        uid = [0]

        def select_block(bk):
            t0 = bk * NT
            for hp in range(16):
                R.dma("sp", lambda e, hp=hp: e.dma_start(out=wqs, in_=d_wq[:, :, hp * 128:(hp + 1) * 128]), b_wqs,
                      writes=[b_wqs])
                R.op("pool", lambda e: e.tensor_copy(out=wqb, in_=wqs), reads=[b_wqs], writes=[b_wqb])
                ps, bps = next_pp()
                for c in range(8):
                    R.op("pe", lambda e, c=c, ps=ps: e.matmul(ps[:, 0:NT], lhsT=wqb[:, c, :],
                                                              rhs=xn2T[:, c, t0:t0 + NT], start=(c == 0),
                                                              stop=(c == 7)), reads=[b_wqb, b_xn2T], writes=[bps])
                R.op("act", lambda e, hp=hp, ps=ps: e.activation(out=qpT[:, hp, :], in_=ps[:, 0:NT], func=AF.Copy),
                     reads=[bps], writes=[b_qpT])
            for tt in range(TB):
                select_tile(tt)

        def select_tile(tt):
            sl, bsl, sm, bsm = sel[tt], b_sel[tt], ssm[tt], b_ssm[tt]
            for g in range(4):
                ps, bps = next_pp()
                for i in range(4):
                    hp = g * 4 + i
                    R.op("pe", lambda e, i=i, hp=hp, ps=ps: e.matmul(
                        ps[:, i * 128:(i + 1) * 128], lhsT=qpT[:, hp, tt * 128:(tt + 1) * 128], rhs=skT[:, hp, :],
                        start=True, stop=True, skip_group_check=True), reads=[b_qpT, b_skT], writes=[bps])
                R.op("act", lambda e, g=g, ps=ps: e.activation(
                    out=sc[:, g * 4:(g + 1) * 4, :], in_=ps.rearrange("p (a n) -> p a n", n=128), func=AF.Copy),
                     reads=[bps], writes=[b_sc])
            for hp in range(16):
                R.op("dve", lambda e, hp=hp: e.max(out=vtop[:, hp, 0:8], in_=sc[:, hp, :]), reads=[b_sc],
                     writes=[b_vtop])
                R.op("dve", lambda e, hp=hp: e.match_replace(out=mtmp[:, 0:128], in_to_replace=vtop[:, hp, 0:8],
                                                             in_values=sc[:, hp, :], imm_value=NEG),
                     reads=[b_sc, b_vtop], writes=[b_mtmp])
                R.op("dve", lambda e, hp=hp: e.max(out=vtop[:, hp, 8:16], in_=mtmp[:, 0:128]), reads=[b_mtmp],
                     writes=[b_vtop])
            vv = vtop.rearrange("p (h two) k -> p h two k", two=2)
            cand4 = cand.rearrange("p h (i j) -> p h i j", j=16)
            R.op("dve", lambda e: e.tensor_tensor(
                out=cand4, in0=vv[:, :, 0, :].unsqueeze(3).to_broadcast([128, 8, 16, 16]),
                in1=vv[:, :, 1, :].unsqueeze(2).to_broadcast([128, 8, 16, 16]), op=ALU.add),
                 reads=[b_vtop], writes=[b_cand])
            for h in range(8):
                R.op("dve", lambda e, h=h: e.max(out=top[:, h, 0:8], in_=cand[:, h, :]), reads=[b_cand],
                     writes=[b_top])
                R.op("dve", lambda e, h=h: e.match_replace(out=mtmp, in_to_replace=top[:, h, 0:8],
                                                           in_values=cand[:, h, :], imm_value=NEG),
                     reads=[b_cand, b_top], writes=[b_mtmp])
                R.op("dve", lambda e, h=h: e.max(out=top[:, h, 8:16], in_=mtmp), reads=[b_mtmp], writes=[b_top])
                R.op("dve", lambda e, h=h: e.match_replace(out=mtmp, in_to_replace=top[:, h, 8:16],
                                                           in_values=mtmp, imm_value=NEG),
                     reads=[b_mtmp, b_top], writes=[b_mtmp])
                R.op("dve", lambda e, h=h: e.max(out=top[:, h, 16:24], in_=mtmp), reads=[b_mtmp], writes=[b_top])
            mx = top[:, :, 0]
            R.op("dve", lambda e: e.tensor_tensor(out=d16, in0=top[:, :, 0:16],
                                                  in1=top[:, :, 0:1].to_broadcast([128, 8, 16]), op=ALU.subtract),
                 reads=[b_top], writes=[b_d16])
            R.op("act", lambda e: e.activation(out=d16, in_=d16, func=AF.Exp), reads=[b_d16], writes=[b_d16])
            R.op("dve", lambda e: e.reduce_sum(out=sm[:, 8:16], in_=d16, axis=AX.X), reads=[b_d16], writes=[bsm])
            R.op("dve", lambda e: e.reciprocal(out=sm[:, 8:16], in_=sm[:, 8:16]), reads=[bsm], writes=[bsm])
            R.op("dve", lambda e: e.tensor_tensor(out=sm[:, 16:24], in0=top[:, :, 15], in1=top[:, :, 16], op=ALU.add),
                 reads=[b_top], writes=[bsm])
            R.op("dve", lambda e: e.scalar_tensor_tensor(out=sm[:, 16:24], in0=sm[:, 16:24], scalar=0.5, in1=mx,
                                                         op0=ALU.mult, op1=ALU.subtract),
                 reads=[bsm, b_top], writes=[bsm])
            R.op("act", lambda e: e.activation(out=sm[:, 16:24], in_=sm[:, 16:24], func=AF.Exp), reads=[bsm],
                 writes=[bsm])
            R.op("dve", lambda e: e.tensor_tensor(out=sm[:, 0:8], in0=sm[:, 16:24], in1=sm[:, 8:16], op=ALU.mult),
                 reads=[bsm], writes=[bsm])
            R.op("dve", lambda e: e.reciprocal(out=sm[:, 24:32], in_=sm[:, 16:24]), reads=[bsm], writes=[bsm])
            R.op("dve", lambda e: e.tensor_tensor(out=sl, in0=sc, in1=vtop[:, :, 0:1].to_broadcast([128, 16, 128]),
                                                  op=ALU.subtract), reads=[b_sc, b_vtop], writes=[bsl])
            R.op("act", lambda e: e.activation(out=sl, in_=sl, func=AF.Exp), reads=[bsl], writes=[bsl])
            slv = sl.rearrange("p (h two) n -> p h two n", two=2)
            R.op("dve", lambda e: e.tensor_tensor(out=slv[:, :, 0, :], in0=slv[:, :, 0, :],
                                                  in1=sm[:, 24:32].unsqueeze(2).to_broadcast([128, 8, 128]),
                                                  op=ALU.mult), reads=[bsl, bsm], writes=[bsl])
            for h in range(8):
                R.op("dve", lambda e, h=h: e.tensor_scalar(out=Dg[tt][:, h, :], in0=ident, scalar1=sm[:, h:h + 1],
                                                           scalar2=None, op0=ALU.mult),
                     reads=[b_cbf, bsm], writes=[b_Dg[tt]])

        def load_chunk(j):
            k = j % 2
            R.dma("sp", lambda e: e.dma_start(out=ust, in_=d_uT[:, :, j * 512:(j + 1) * 512]), b_ust, writes=[b_ust])
            R.op("act", lambda e: e.activation(out=ubf[k], in_=ust, func=AF.Copy), reads=[b_ust], writes=[b_ubf[k]])
            R.dma("sp", lambda e: e.dma_start(
                out=vst, in_=d_ev[j * 512:(j + 1) * 512, :].rearrange("(a p) d -> p a d", p=128)), b_vst,
                  writes=[b_vst])
            R.op("act", lambda e: e.activation(out=vbf[k], in_=vst, func=AF.Copy), reads=[b_vst], writes=[b_vbf[k]])

        def chunk_gelu(bk, j):
            k = j % 2
            t0 = bk * NT
            for a in range(4):
                u = uid[0]
                uid[0] += 1
                psA, bA = ps_S[u % 2][:], bS[u % 2]
                for c in range(8):
                    R.op("pe", lambda e, c=c, a=a, psA=psA: e.matmul(
                        psA[:, 0:NT], lhsT=ubf[k][:, c, a * 128:(a + 1) * 128], rhs=xn2T[:, c, t0:t0 + NT],
                        start=(c == 0), stop=(c == 7)), reads=[b_ubf[k], b_xn2T], writes=[bA])
                R.op("act", lambda e, a=a, psA=psA: e.activation(out=gaT[k][:, a, :], in_=psA[:, 0:NT], func=AF.Gelu),
                     reads=[bA], writes=[b_gaT[k]])

        b_Mbh = [Buf("Mb%d" % i) for i in range(2)]
        b_Eb1a, b_Eb1b = Buf("Eb1a"), Buf("Eb1b")

        def gates_E(tt, j):
            slv = sel[tt].rearrange("p (h two) n -> p h two n", two=2)
            R.op("dve", lambda e: e.tensor_tensor(
                out=Eb[0].rearrange("p h (a n) -> p h a n", n=128),
                in0=slv[:, 0:4, 0, 4 * j:4 * j + 4].unsqueeze(3).to_broadcast([128, 4, 4, 128]),
                in1=slv[:, 0:4, 1, :].unsqueeze(2).to_broadcast([128, 4, 4, 128]), op=ALU.mult),
                 reads=[b_sel[tt]], writes=[b_Eb[0]])
            E1v = Eb[1].rearrange("p h (a n) -> p h a n", n=128)
            R.op("dve", lambda e: e.tensor_tensor(
                out=E1v[:, 0:2],
                in0=slv[:, 4:6, 0, 4 * j:4 * j + 4].unsqueeze(3).to_broadcast([128, 2, 4, 128]),
                in1=slv[:, 4:6, 1, :].unsqueeze(2).to_broadcast([128, 2, 4, 128]), op=ALU.mult),
                 reads=[b_sel[tt]], writes=[b_Eb1a])
            for h in range(2, 4):
                for a in range(4):
                    R.op("act", lambda e, h=h, a=a: e.activation(
                        out=E1v[:, h, a, :], in_=slv[:, 4 + h, 1, :], func=AF.Copy,
                        scale=slv[:, 4 + h, 0, 4 * j + a:4 * j + a + 1]), reads=[b_sel[tt]], writes=[b_Eb1b])

        def gates_M(tt, j):
            for hh in range(2):
                R.op("dve", lambda e, hh=hh: e.scalar_tensor_tensor(
                    out=Mb[:, hh * 4:(hh + 1) * 4, :], in0=Eb[hh], scalar=1.0, in1=Eb[hh], op0=ALU.is_ge,
                    op1=ALU.mult), reads=([b_Eb[0]] if hh == 0 else [b_Eb1a, b_Eb1b]), writes=[b_Mbh[hh]])

        def transposes(tt, j):
            gp, bgp = next_pp()
            for h in range(8):
                for a in range(4):
                    R.op("pe", lambda e, a=a, h=h: e.matmul(
                        gp[:, a * 128:(a + 1) * 128], lhsT=Mb[:, h, a * 128:(a + 1) * 128], rhs=Dg[tt][:, h, :],
                        start=(h == 0 and a == 0), stop=(h == 7), skip_group_check=True),
                         reads=[b_Mbh[h // 4], b_Dg[tt]], writes=[bgp])
            return gp, bgp

        def make_HT(tt, j, u, gp, bgp):
            k = j % 2
            htu, bhtu = HT[u % 2], b_HT[u % 2]
            R.op("dve", lambda e: e.tensor_tensor(out=htu, in0=gp.rearrange("p (a t) -> p a t", t=128),
                                                  in1=gaT[k][:, :, tt * 128:(tt + 1) * 128], op=ALU.mult),
                 reads=[bgp, b_gaT[k]], writes=[bhtu])

        def final(tt, j, u):
            k = j % 2
            htu, bhtu = HT[u % 2], b_HT[u % 2]
            psY, bY = ps_O[u % 2][:], bO[u % 2]
            for hf in range(2):
                for a in range(4):
                    R.op("pe", lambda e, a=a, hf=hf: e.matmul(
                        psY[:, hf * 512:(hf + 1) * 512], lhsT=htu[:, a, :], rhs=vbf[k][:, a, hf * 512:(hf + 1) * 512],
                        start=(a == 0), stop=(a == 3)), reads=[bhtu, b_vbf[k]], writes=[bY])
            if j == 0:
                R.op("act", lambda e: e.activation(out=oacc[:, tt, :], in_=psY, func=AF.Copy), reads=[bY],
                     writes=[b_oacc[tt]])
            else:
                R.op("act", lambda e: e.activation(out=ytmp, in_=psY, func=AF.Copy), reads=[bY], writes=[b_ytmp])
                R.op("pool", lambda e: e.tensor_tensor(out=oacc[:, tt, :], in0=oacc[:, tt, :], in1=ytmp, op=ALU.add),
                     reads=[b_ytmp, b_oacc[tt]], writes=[b_oacc[tt]])

        NCH = NE // 512
        for bk in range(NBK):
            R.barrier()
            select_block(bk)
            R.barrier()
            load_chunk(0)
            chunk_gelu(bk, 0)
            ulist = [(j, tt) for j in range(NCH) for tt in range(TB)]
            gates_E(ulist[0][1], ulist[0][0])
            gates_M(ulist[0][1], ulist[0][0])
            for i, (j, tt) in enumerate(ulist):
                u = uid[0]
                uid[0] += 1
                if tt == 0 and j + 1 < NCH:
                    load_chunk(j + 1)
                gp, bgp = transposes(tt, j)
                nxt = ulist[i + 1] if i + 1 < len(ulist) else None
                if nxt is not None:
                    gates_E(nxt[1], nxt[0])
                make_HT(tt, j, u, gp, bgp)
                if nxt is not None:
                    gates_M(nxt[1], nxt[0])
                final(tt, j, u)
                if tt == min(1, TB - 1) and j + 1 < NCH:
                    chunk_gelu(bk, j + 1)
            R.barrier()
            for tt in range(TB):
                qi = bk * TB + tt

                def epilogue(tt=tt, qi=qi):
                    R.dma("sp", lambda e: e.dma_start(out=hb2, in_=d_hs[qi * 128:(qi + 1) * 128, :]), b_hb2,
                          reads=[b_hs[qi]], writes=[b_hb2])
                    R.op("dve", lambda e: e.tensor_tensor(out=hb2, in0=hb2, in1=oacc[:, tt, :], op=ALU.add),
                         reads=[b_hb2, b_oacc[tt]], writes=[b_hb2])
                    final_norm(hb2, b_hb2, jk2, b_jk2, st2, b_st2, ob2, b_ob2, qi)
                epilogue()

    counts = R.finish(nc, stack)
    stack.close()
    print('[kernel] arena peak words', A.peak, 'of', ARENA_WORDS, 'sem incs', counts)
    return nc, counts, A.peak


def _chunk_rows(w):
    n = w.shape[1]
    return np.ascontiguousarray(w.reshape(8, 128, n).transpose(1, 0, 2))


def _tables(S):
    f32 = np.float32
    t = np.arange(S)
    row = (t // 64).astype(f32)
    col = (t % 64).astype(f32)
    pos = t.astype(f32)
    inv_ax = (f32(10000.0) ** (-np.arange(0, 32, 2, dtype=f32) / f32(32))).astype(f32)
    inv_p = (f32(500000.0) ** (-np.arange(0, 16, 2, dtype=f32) / f32(16))).astype(f32)
    ra = (row[:, None] * inv_ax[None, :]).astype(f32)
    ca = (col[:, None] * inv_ax[None, :]).astype(f32)
    pa = (pos[:, None] * inv_p[None, :]).astype(f32)
    cA = np.concatenate([np.cos(ra), np.cos(ra), np.cos(ca), np.cos(ca)], axis=1).T
    sA = np.concatenate([-np.sin(ra), np.sin(ra), -np.sin(ca), np.sin(ca)], axis=1).T
    cB = np.concatenate([np.cos(pa), np.cos(pa), np.ones((S, 48), f32)], axis=1).T
    sB = np.concatenate([-np.sin(pa), np.sin(pa), np.zeros((S, 48), f32)], axis=1).T
    tabA = np.stack([np.tile(cA, (2, 1)), np.tile(sA, (2, 1))], axis=1).astype(f32)
    tabB = np.stack([np.tile(cB, (2, 1)), np.tile(sB, (2, 1))], axis=1).astype(f32)
    return np.ascontiguousarray(tabA), np.ascontiguousarray(tabB)


def _consts():
    ident = np.eye(128, dtype=np.float32)
    ones = np.ones((128, 128), np.float32)
    blk = np.zeros((128, 128), np.float32)
    blk[:64, :64] = 1
    blk[64:, 64:] = 1

    def perm(partner):
        p = np.zeros((128, 128), np.float32)
        for m in range(128):
            p[(m // 64) * 64 + partner(m % 64), m] = 1
        return p

    def pa(d):
        return d + 16 if (d % 32) < 16 else d - 16

    def pb(d):
        if d < 8:
            return d + 8
        if d < 16:
            return d - 8
        return d

    return np.ascontiguousarray(np.stack([ident, ones, blk, perm(pa), perm(pb)], axis=1))


_CACHE = {}


def kernel(x, norm_attn_g, w_in, q_norm_g, k_norm_g, lambda_q1, lambda_k1, lambda_q2, lambda_k2, subln_g,
           w_out, norm_ffn_g, w_query, sub_keys, expert_u, expert_v, norm_final_g, _with_peer=True):
    f32 = np.float32
    x = np.asarray(x, f32)
    S = x.shape[1]
    TQ = S // NCORE
    x2 = x[0]
    xT = np.ascontiguousarray(x2.T.reshape(8, 128, S).transpose(1, 0, 2))
    w = np.asarray(w_in, f32)[0]
    qa, ka, va = w[:, 0:512], w[:, 512:640], w[:, 640:768]
    qb, kb, vb = w[:, 768:1280], w[:, 1280:1792], w[:, 1792:2304]
    qa_p = np.concatenate([np.concatenate([qa[:, j * 64:(j + 1) * 64], qa[:, (j + 4) * 64:(j + 5) * 64]], axis=1)
                           for j in range(4)], axis=1)
    wA = _chunk_rows(np.concatenate([qa_p, ka, va], axis=1))
    wB = _chunk_rows(np.concatenate([qb, kb, vb], axis=1))
    wout = _chunk_rows(np.asarray(w_out, f32)[0])
    tabA, tabB = _tables(S)
    vec = np.zeros((128, 18), f32)
    vec[:, 0:8] = np.asarray(norm_attn_g, f32)[0].reshape(8, 128).T
    vec[:, 8:16] = np.asarray(norm_ffn_g, f32)[0].reshape(8, 128).T
    vec[:, 16] = np.tile(np.asarray(q_norm_g, f32)[0], 2)
    vec[:, 17] = np.tile(np.asarray(k_norm_g, f32)[0], 2)
    lam = np.stack([np.asarray(a, f32)[0] for a in (lambda_q1, lambda_k1, lambda_q2, lambda_k2)], axis=0)
    lam = np.ascontiguousarray(np.broadcast_to(lam[None], (128, 4, 64)))
    sub = np.ascontiguousarray(np.broadcast_to(np.asarray(subln_g, f32)[0][None], (128, 128)))
    gfin = np.ascontiguousarray(np.broadcast_to(np.asarray(norm_final_g, f32)[None], (128, D)))
    gffn = np.ascontiguousarray(np.broadcast_to(np.asarray(norm_ffn_g, f32)[0][None], (128, D)))
    cst = _consts()
    key = (S, _with_peer)
    if key not in _CACHE:
        _CACHE[key] = build(S, _with_peer)
    nc, counts, peak = _CACHE[key]
    common = dict(xT=xT, wA=wA, wB=wB, wout=wout, cst=cst, tabA=tabA, tabB=tabB, vec=vec, lam=lam, subln=sub,
                  gfin=gfin, gffn=gffn)
    if _with_peer:
        common["wq"] = _chunk_rows(np.asarray(w_query, f32)[0])
        sk = np.asarray(sub_keys, f32)[0].reshape(16, 128, 128)
        common["skT"] = np.ascontiguousarray(sk.transpose(2, 0, 1))
        common["uT"] = _chunk_rows(np.ascontiguousarray(np.asarray(expert_u, f32)[0].T))
        common["ev"] = np.ascontiguousarray(np.asarray(expert_v, f32)[0])
    in_maps = []
    for c in range(NCORE):
        sl = slice(c * TQ, (c + 1) * TQ)
        m = dict(common)
        m["xTo"] = np.ascontiguousarray(xT[:, :, sl])
        m["xo"] = np.ascontiguousarray(x2[sl])
        m["tabAo"] = np.ascontiguousarray(tabA[:, :, sl])
        m["tabBo"] = np.ascontiguousarray(tabB[:, :, sl])
        in_maps.append(m)
    res = run_bass_kernel_spmd(nc, in_maps, core_ids=list(range(NCORE)))
    out = np.concatenate([np.asarray(r["out"], f32) for r in res.results], axis=0)
    return out.reshape(1, S, D)
```

```python
import math
from contextlib import ExitStack

import numpy as np
import concourse.bass as bass
import concourse.mybir as mybir
from concourse.bass_utils import run_bass_kernel_spmd

F32 = mybir.dt.float32
BF16 = mybir.dt.bfloat16
ALU = mybir.AluOpType
AF = mybir.ActivationFunctionType
AX = mybir.AxisListType

NCORE = 8
D = 1024
NORM_EPS = 1e-6
SUBLN_EPS = 1e-5
LAMBDA_INIT = 0.8 - 0.6 * math.exp(-0.3 * 0)
ENGS = ["pe", "act", "dve", "pool", "sp"]


class Buf:
    __slots__ = ("name", "w", "r", "dcnt", "sem", "uses_dma", "excl")

    def __init__(self, name, excl=False):
        self.name = name
        self.excl = excl
        self.w = None
        self.r = {}
        self.dcnt = 0
        self.sem = None
        self.uses_dma = False


class Op:
    __slots__ = ("eng", "fn", "deps", "tok", "dma", "waits", "inc")

    def __init__(self, eng, fn, deps, tok, dma):
        self.eng = eng
        self.fn = fn
        self.deps = deps
        self.tok = tok
        self.dma = dma
        self.waits = []
        self.inc = False


class Rec:
    def __init__(self):
        self.ops = []
        self.cnt = {e: 0 for e in ENGS}
        self.dma_bufs = []
        self.pending = {e: set() for e in ENGS}

    def _deps(self, eng, reads, writes):
        deps = set()
        for b in reads:
            if b.w is not None:
                if not (b.w[0] == "e" and b.w[1] == eng and eng == "pe"):
                    deps.add(b.w)
            if b.excl:
                for t in b.r.values():
                    if not (t[0] == "e" and t[1] == eng):
                        deps.add(t)
        for b in writes:
            if b.w is not None and not (b.w[0] == "e" and b.w[1] == eng):
                deps.add(b.w)
            for t in b.r.values():
                if not (t[0] == "e" and t[1] == eng):
                    deps.add(t)
        if self.pending[eng]:
            deps |= self.pending[eng]
            self.pending[eng] = set()
        return deps

    def op(self, eng, fn, reads=(), writes=()):
        deps = self._deps(eng, reads, writes)
        self.cnt[eng] += 1
        tok = ("e", eng, self.cnt[eng])
        self.ops.append(Op(eng, fn, deps, tok, None))
        for b in writes:
            b.w = tok
            b.r = {}
        for b in reads:
            b.r[eng] = tok
        return tok

    def dma(self, eng, fn, anchor, reads=(), writes=()):
        deps = self._deps(eng, reads, writes)
        if not anchor.uses_dma:
            anchor.uses_dma = True
            self.dma_bufs.append(anchor)
        anchor.dcnt += 16
        tok = ("d", anchor, anchor.dcnt)
        self.ops.append(Op(eng, fn, deps, tok, anchor))
        for b in writes:
            b.w = tok
            b.r = {}
        for b in reads:
            b.r[("d", anchor)] = tok
        return tok

    def barrier(self):
        toks = set()
        for e in ENGS:
            if e != "sp" and self.cnt[e] > 0:
                toks.add(("e", e, self.cnt[e]))
        for b in self.dma_bufs:
            toks.add(("d", b, b.dcnt))
        for e in ENGS:
            self.pending[e] |= {t for t in toks if not (t[0] == "e" and t[1] == e)}

    def finish(self, nc, stack):
        self.barrier()
        self.ops.append(Op("sp", None, self.pending["sp"], None, None))
        self.pending["sp"] = set()
        needed = set()
        waited = {e: {} for e in ENGS}
        for op in self.ops:
            w = waited[op.eng]
            for d in sorted(op.deps, key=lambda t: (t[0], str(t[1] if t[0] == "e" else t[1].name), t[2])):
                key = d[1]
                if w.get(key, 0) < d[2]:
                    w[key] = d[2]
            per = {}
            for d in op.deps:
                key = d[1]
                if key not in per or per[key][2] < d[2]:
                    per[key] = d
            op.waits = list(per.values())
        waited = {e: {} for e in ENGS}
        for op in self.ops:
            w = waited[op.eng]
            keep = []
            for d in op.waits:
                key = d[1]
                if w.get(key, 0) < d[2]:
                    w[key] = d[2]
                    keep.append(d)
                    if d[0] == "e":
                        needed.add((d[1], d[2]))
            op.waits = keep
        incval = {}
        c = {e: 0 for e in ENGS}
        for op in self.ops:
            if op.tok is not None and op.tok[0] == "e" and (op.tok[1], op.tok[2]) in needed:
                c[op.eng] += 1
                incval[(op.tok[1], op.tok[2])] = c[op.eng]
                op.inc = True
        esem = {e: stack.enter_context(nc.semaphore("es_" + e)) for e in ENGS if e != "sp"}
        for b in self.dma_bufs:
            b.sem = stack.enter_context(nc.semaphore("ds_" + b.name))
        per_eng = {e: [o for o in self.ops if o.eng == e] for e in ENGS}

        def emit(engobj, ename):
            for op in per_eng[ename]:
                for d in op.waits:
                    if d[0] == "e":
                        engobj.wait_ge(esem[d[1]], incval[(d[1], d[2])])
                    else:
                        engobj.wait_ge(d[1].sem, d[2])
                if op.fn is None:
                    continue
                ins = op.fn(engobj)
                if op.dma is not None:
                    ins.then_inc(op.dma.sem, 16)
                elif op.inc:
                    ins.then_inc(esem[ename], 1)

        with nc.Block() as block:
            @block.sync
            def _(e):
                emit(e, "sp")

            @block.tensor
            def _(e):
                emit(e, "pe")

            @block.scalar
            def _(e):
                emit(e, "act")

            @block.vector
            def _(e):
                emit(e, "dve")

            @block.gpsimd
            def _(e):
                emit(e, "pool")
        return {e: c[e] for e in ENGS}


class Arena:
    def __init__(self, ap, nwords):
        self.ap = ap
        self.n = nwords
        self.off = 0
        self.peak = 0

    def alloc(self, shape, dtype, top=False):
        n = int(np.prod(shape))
        words = n if dtype == F32 else (n + 1) // 2
        if top:
            self.n -= words
            a = self.ap[:, self.n:self.n + words]
        else:
            a = self.ap[:, self.off:self.off + words]
            self.off += words
        self.peak = max(self.peak, self.off)
        assert self.off <= self.n, ("arena overflow", self.off, self.n)
        if dtype != F32:
            a = a.bitcast(dtype)
        if len(shape) == 1:
            return a
        if len(shape) == 2:
            return a.rearrange("p (a b) -> p a b", b=shape[1])
        if len(shape) == 3:
            return a.rearrange("p (a b c) -> p a b c", b=shape[1], c=shape[2])
        if len(shape) == 4:
            return a.rearrange("p (a b c d) -> p a b c d", b=shape[1], c=shape[2], d=shape[3])
        raise ValueError(shape)


def build(S, with_peer=True):
    TQ = S // NCORE
    QB = min(512, TQ)
    NQB = TQ // QB
    NQC = QB // 128
    NQI = TQ // 128
    KB = 512
    NKB = S // KB
    NE = 16384
    nc = bass.Bass("TRN2", target_bir_lowering=False)
    R = Rec()

    def din(name, shape):
        return nc.dram_tensor(name, list(shape), F32, kind="ExternalInput").ap()

    d_xT = din("xT", [128, 8, S])
    d_xTo = din("xTo", [128, 8, TQ])
    d_xo = din("xo", [TQ, D])
    d_wA = din("wA", [128, 8, 768])
    d_wB = din("wB", [128, 8, 1536])
    d_wout = din("wout", [128, 8, D])
    d_cst = din("cst", [128, 5, 128])
    d_tabA = din("tabA", [128, 2, S])
    d_tabB = din("tabB", [128, 2, S])
    d_tabAo = din("tabAo", [128, 2, TQ])
    d_tabBo = din("tabBo", [128, 2, TQ])
    d_vec = din("vec", [128, 8 + 8 + 2])
    d_lam = din("lam", [128, 4, 64])
    d_sub = din("subln", [128, 128])
    d_gfin = din("gfin", [128, D])
    d_gffn = din("gffn", [128, D])
    if with_peer:
        d_wq = din("wq", [128, 8, 2048])
        d_skT = din("skT", [128, 16, 128])
        d_uT = din("uT", [128, 8, NE])
        d_ev = din("ev", [NE, D])
        d_hs = nc.dram_tensor("hscr", [TQ, D], F32, kind="Internal").ap()
    d_out = nc.dram_tensor("out", [TQ, D], F32, kind="ExternalOutput").ap()

    stack = ExitStack()
    ARENA_WORDS = 53200
    arena_t = stack.enter_context(nc.sbuf_tensor("arena", [128, ARENA_WORDS], F32))
    A = Arena(arena_t[:], ARENA_WORDS)
    ps_S = [stack.enter_context(nc.psum_tensor("psS%d" % i, [128, 512], F32)) for i in range(2)]
    ps_O = [stack.enter_context(nc.psum_tensor("psO%d" % i, [128, 1024], F32)) for i in range(2)]
    ps_P = [stack.enter_context(nc.psum_tensor("psP%d" % i, [128, 512], F32)) for i in range(2)]
    bS = [Buf("psS%d" % i, True) for i in range(2)]
    bO = [Buf("psO%d" % i, True) for i in range(2)]
    bP = [Buf("psP%d" % i, True) for i in range(2)]
    pp_i = [0]

    def next_pp():
        i = pp_i[0] % 2
        pp_i[0] += 1
        return ps_P[i][:], bP[i]

    cst_f = A.alloc([5, 128], F32)
    cst = A.alloc([5, 128], BF16)
    vec = A.alloc([18], F32)
    lam4 = A.alloc([4, 64], F32)
    subg = A.alloc([128], F32)
    gfin = A.alloc([D], F32)
    small = A.alloc([64], F32)
    b_cst = Buf("cst")
    b_small = Buf("small")
    ident = cst[:, 0, :]
    ones_bf = cst[:, 1, :]
    onesblk = cst[:, 2, :]
    permA = cst[:, 3, :]
    permB = cst[:, 4, :]
    Otok = A.alloc([NQI, D], BF16, top=True)
    b_Otok = [Buf("Otok%d" % i) for i in range(NQI)]

    R.dma("sp", lambda e: e.dma_start(out=cst_f, in_=d_cst), b_cst, writes=[b_cst])
    R.dma("sp", lambda e: e.dma_start(out=vec, in_=d_vec), b_cst, writes=[b_cst])
    R.dma("sp", lambda e: e.dma_start(out=lam4, in_=d_lam), b_cst, writes=[b_cst])
    R.dma("sp", lambda e: e.dma_start(out=subg, in_=d_sub), b_cst, writes=[b_cst])
    R.dma("sp", lambda e: e.dma_start(out=gfin, in_=d_gfin), b_cst, writes=[b_cst])
    b_cbf = Buf("cbf")
    R.op("dve", lambda e: e.tensor_copy(out=cst, in_=cst_f), reads=[b_cst], writes=[b_cbf])
    lt = A.alloc([2, 64], F32)
    R.op("dve", lambda e: e.tensor_tensor(out=lt[:, 0, :], in0=lam4[:, 0, :], in1=lam4[:, 1, :], op=ALU.mult),
         reads=[b_cst], writes=[b_small])
    R.op("dve", lambda e: e.tensor_tensor(out=lt[:, 1, :], in0=lam4[:, 2, :], in1=lam4[:, 3, :], op=ALU.mult),
         reads=[b_cst, b_small], writes=[b_small])
    R.op("dve", lambda e: e.reduce_sum(out=small[:, 2:4], in_=lt, axis=AX.X), reads=[b_small], writes=[b_small])
    R.op("act", lambda e: e.activation(out=small[:, 4:6], in_=small[:, 2:4], func=AF.Exp),
         reads=[b_small], writes=[b_small])
    R.op("dve", lambda e: e.tensor_tensor(out=small[:, 0:1], in0=small[:, 4:5], in1=small[:, 5:6], op=ALU.subtract),
         reads=[b_small], writes=[b_small])
    R.op("dve", lambda e: e.tensor_scalar(out=small[:, 0:1], in0=small[:, 0:1], scalar1=float(LAMBDA_INIT),
                                          scalar2=None, op0=ALU.add), reads=[b_small], writes=[b_small])
    R.op("dve", lambda e: e.tensor_scalar(out=subg, in0=subg, scalar1=float(1.0 - LAMBDA_INIT), scalar2=None,
                                          op0=ALU.mult), reads=[b_cst], writes=[b_cst])
    lam = small[:, 0:1]

    def rsqrt_mean(eng, out, in_, n, eps, reads, writes):
        R.op(eng, lambda e: e.tensor_scalar(out=out, in0=in_, scalar1=1.0 / n, scalar2=float(eps),
                                            op0=ALU.mult, op1=ALU.add), reads=reads, writes=writes)
        R.op("act", lambda e: e.activation(out=out, in_=out, func=AF.Ln), reads=writes, writes=writes)
        R.op("act", lambda e: e.activation(out=out, in_=out, func=AF.Exp, scale=-0.5), reads=writes, writes=writes)

    phase_mark = A.off

    def attn_phase(tag, d_w, ncol_q, nkc, vdim, ngroups, headnorm, d_tab, d_tabo, perm, maps, ocol0, diff):
        A.off = phase_mark
        R.barrier()
        ncol = ncol_q + nkc * 128 + ngroups * vdim
        kcol0 = ncol_q
        vcol0 = ncol_q + nkc * 128
        nqch = ncol_q // 128
        vd1 = vdim + 1
        nmap = len(maps)
        wbf = A.alloc([8, ncol], BF16)
        b_w = Buf(tag + "w")
        QT = A.alloc([nqch, TQ], BF16)
        b_QT = Buf(tag + "QT")
        acc = A.alloc([nmap, NQI, vd1], F32)
        b_acc = Buf(tag + "acc")
        xTb = A.alloc([8, 256], F32)
        b_xT = Buf(tag + "xT")
        sq = A.alloc([8, 256], BF16)
        b_sq = Buf(tag + "sq")
        rstd = A.alloc([256], F32)
        b_rstd = Buf(tag + "rstd")
        xn = A.alloc([8, 512], BF16)
        b_xn = Buf(tag + "xn")
        tab = A.alloc([2, 512], F32)
        b_tab = Buf(tag + "tab")
        KT = [A.alloc([nkc, 512], BF16) for _ in range(2)]
        b_KT = [Buf(tag + "KT%d" % i) for i in range(2)]
        Vt_flat = [A.alloc([4 * ngroups * (vdim + 2)], BF16) for _ in range(2)]
        Vt = [v.rearrange("p (t g v) -> p t g v", g=ngroups, v=vdim + 2) for v in Vt_flat]
        b_Vt = [Buf(tag + "Vt%d" % i) for i in range(2)]
        Pb = [A.alloc([512], BF16) for _ in range(3)]
        b_Pb = [Buf(tag + "P%d" % i) for i in range(3)]
        t_sq = A.alloc([512], BF16)
        t_rs = A.alloc([512], F32)
        t_n = A.alloc([512], BF16)
        t_1 = A.alloc([512], F32)
        t_2 = A.alloc([512], F32)
        b_tsq, b_trs, b_tn, b_t1, b_t2 = (Buf(tag + n) for n in ("tsq", "trs", "tn", "t1", "t2"))
        wst, b_wst = t_2, b_t2

        for c in range(8):
            for c0 in range(0, ncol, 512):
                cw = min(512, ncol - c0)
                R.dma("sp", lambda e, c=c, c0=c0, cw=cw: e.dma_start(out=wst[:, 0:cw], in_=d_w[:, c, c0:c0 + cw]),
                      b_wst, writes=[b_wst])
                R.op("dve", lambda e, c=c, c0=c0, cw=cw: e.tensor_scalar(
                    out=wbf[:, c, c0:c0 + cw], in0=wst[:, 0:cw], scalar1=vec[:, c:c + 1], scalar2=None,
                    op0=ALU.mult), reads=[b_wst, b_cst], writes=[b_w])
        for i in range(2):
            R.op("dve", lambda e, i=i: e.memset(Vt_flat[i], 1.0), writes=[b_Vt[i]])

        def norm_unit(src, dst):
            R.dma("sp", lambda e: e.dma_start(out=xTb, in_=src), b_xT, writes=[b_xT])
            R.op("act", lambda e: e.activation(out=sq, in_=xTb, func=AF.Square), reads=[b_xT], writes=[b_sq])
            yield
            ps, bps = next_pp()
            for c in range(8):
                R.op("pe", lambda e, c=c: e.matmul(ps[:, 0:256], lhsT=ones_bf, rhs=sq[:, c, :], start=(c == 0),
                                                   stop=(c == 7)), reads=[b_sq, b_cbf], writes=[bps])
            rsqrt_mean("dve", rstd, ps[:, 0:256], float(D), NORM_EPS, [bps], [b_rstd])
            R.op("dve", lambda e: e.tensor_tensor(out=dst, in0=xTb,
                                                  in1=rstd.unsqueeze(1).to_broadcast([128, 8, 256]), op=ALU.mult),
                 reads=[b_xT, b_rstd], writes=[b_xn])
            yield

        def run(gen):
            for _ in gen:
                pass

        def load_tab(src):
            R.dma("sp", lambda e: e.dma_start(out=tab, in_=src), b_tab, writes=[b_tab])

        def proj_post(wcol, n, gcol, dst, b_dst):
            ps, bps = next_pp()
            for c in range(8):
                R.op("pe", lambda e, c=c: e.matmul(ps[:, 0:n], lhsT=wbf[:, c, wcol:wcol + 128], rhs=xn[:, c, 0:n],
                                                   start=(c == 0), stop=(c == 7)), reads=[b_w, b_xn], writes=[bps])
            if headnorm:
                R.op("act", lambda e: e.activation(out=t_sq[:, 0:n], in_=ps[:, 0:n], func=AF.Square),
                     reads=[bps], writes=[b_tsq])
                yield
                ps2, bps2 = next_pp()
                R.op("pe", lambda e: e.matmul(ps2[:, 0:n], lhsT=onesblk, rhs=t_sq[:, 0:n], start=True, stop=True),
                     reads=[b_tsq, b_cbf], writes=[bps2])
                rsqrt_mean("dve", t_rs[:, 0:n], ps2[:, 0:n], 64.0, NORM_EPS, [bps2], [b_trs])
                R.op("dve", lambda e: e.scalar_tensor_tensor(out=t_1[:, 0:n], in0=ps[:, 0:n],
                                                             scalar=vec[:, gcol:gcol + 1], in1=t_rs[:, 0:n],
                                                             op0=ALU.mult, op1=ALU.mult),
                     reads=[bps, b_trs, b_cst], writes=[b_t1])
                R.op("pool", lambda e: e.tensor_copy(out=t_n[:, 0:n], in_=t_1[:, 0:n]), reads=[b_t1], writes=[b_tn])
                yield
                ps3, bps3 = bps2 and (ps2, bps2)
                R.op("pe", lambda e: e.matmul(ps3[:, 0:n], lhsT=perm, rhs=t_n[:, 0:n], start=True, stop=True),
                     reads=[b_tn, b_cbf], writes=[bps3])
                R.op("pool", lambda e: e.tensor_tensor(out=t_1[:, 0:n], in0=t_1[:, 0:n], in1=tab[:, 0, 0:n],
                                                       op=ALU.mult), reads=[b_t1, b_tab], writes=[b_t1])
            else:
                R.op("act", lambda e: e.activation(out=t_n[:, 0:n], in_=ps[:, 0:n], func=AF.Copy),
                     reads=[bps], writes=[b_tn])
                yield
                ps3, bps3 = next_pp()
                R.op("pe", lambda e: e.matmul(ps3[:, 0:n], lhsT=perm, rhs=t_n[:, 0:n], start=True, stop=True),
                     reads=[b_tn, b_cbf], writes=[bps3])
                R.op("dve", lambda e: e.tensor_tensor(out=t_1[:, 0:n], in0=ps[:, 0:n], in1=tab[:, 0, 0:n],
                                                      op=ALU.mult), reads=[bps, b_tab], writes=[b_t1])
            R.op("dve", lambda e: e.tensor_tensor(out=t_2[:, 0:n], in0=ps3[:, 0:n], in1=tab[:, 1, 0:n], op=ALU.mult),
                 reads=[bps3, b_tab], writes=[b_t2])
            R.op("pool", lambda e: e.tensor_tensor(out=dst, in0=t_1[:, 0:n], in1=t_2[:, 0:n], op=ALU.add),
                 reads=[b_t1, b_t2], writes=[b_dst])
            yield

        for qb in range(NQB):
            for hf in range(QB // 256 if QB >= 256 else 1):
                w_ = min(256, QB)
                t0 = qb * QB + hf * w_
                if w_ == 256:
                    run(norm_unit(d_xTo[:, :, t0:t0 + 256], xn[:, :, hf * 256:(hf + 1) * 256]))
                else:
                    R.dma("sp", lambda e, t0=t0: e.dma_start(out=xTb[:, :, 0:128], in_=d_xTo[:, :, t0:t0 + 128]),
                          b_xT, writes=[b_xT])
                    R.op("act", lambda e: e.activation(out=sq[:, :, 0:128], in_=xTb[:, :, 0:128], func=AF.Square),
                         reads=[b_xT], writes=[b_sq])
                    ps, bps = next_pp()
                    for c in range(8):
                        R.op("pe", lambda e, c=c, ps=ps: e.matmul(ps[:, 0:128], lhsT=ones_bf, rhs=sq[:, c, 0:128],
                                                                  start=(c == 0), stop=(c == 7)),
                             reads=[b_sq, b_cbf], writes=[bps])
                    rsqrt_mean("dve", rstd[:, 0:128], ps[:, 0:128], float(D), NORM_EPS, [bps], [b_rstd])
                    R.op("dve", lambda e: e.tensor_tensor(
                        out=xn[:, :, 0:128], in0=xTb[:, :, 0:128],
                        in1=rstd[:, 0:128].unsqueeze(1).to_broadcast([128, 8, 128]), op=ALU.mult),
                         reads=[b_xT, b_rstd], writes=[b_xn])
            load_tab(d_tabo[:, :, qb * QB:(qb + 1) * QB]) if QB == 512 else R.dma(
                "sp", lambda e, qb=qb: e.dma_start(out=tab[:, :, 0:QB], in_=d_tabo[:, :, qb * QB:(qb + 1) * QB]),
                b_tab, writes=[b_tab])
            for j in range(nqch):
                run(proj_post(j * 128, QB, 16, QT[:, j, qb * QB:(qb + 1) * QB], b_QT))

        def inproj(kb):
            s = kb % 2
            for hf in range(2):
                t0 = kb * KB + hf * 256
                yield from norm_unit(d_xT[:, :, t0:t0 + 256], xn[:, :, hf * 256:(hf + 1) * 256])
            load_tab(d_tab[:, :, kb * KB:(kb + 1) * KB])
            for kc in range(nkc):
                yield from proj_post(kcol0 + kc * 128, 512, 17, KT[s][:, kc, :], b_KT[s])
            for tt in range(4):
                vw = ngroups * vdim
                for c0 in range(0, vw, 512):
                    cw = min(512, vw - c0)
                    ps, bps = next_pp()
                    for c in range(8):
                        R.op("pe", lambda e, c=c, ps=ps, c0=c0, cw=cw, tt=tt: e.matmul(
                            ps[:, 0:cw], lhsT=xn[:, c, tt * 128:(tt + 1) * 128],
                            rhs=wbf[:, c, vcol0 + c0:vcol0 + c0 + cw], start=(c == 0), stop=(c == 7)),
                             reads=[b_xn, b_w], writes=[bps])
                    g0 = c0 // vdim
                    ng = cw // vdim
                    R.op("act", lambda e, ps=ps, tt=tt, g0=g0, ng=ng, cw=cw, s=s: e.activation(
                        out=Vt[s][:, tt, g0:g0 + ng, 0:vdim],
                        in_=ps[:, 0:cw].rearrange("p (g v) -> p g v", v=vdim), func=AF.Copy),
                         reads=[bps], writes=[b_Vt[s]])
                    yield

        def oreg(o_ps, qc):
            if vd1 * NQC <= 512:
                return o_ps[:, qc * vd1:(qc + 1) * vd1]
            bk, i = qc // 2, qc % 2
            return o_ps[:, bk * 512 + i * vd1: bk * 512 + (i + 1) * vd1]

        def emit_qk(st):
            kb, m, qb, u, kt = st
            s = kb % 2
            kc, poff, qch, vg = maps[m]
            si = u * 4 + kt
            sps, bs = ps_S[si % 2][:], bS[si % 2]
            R.op("pe", lambda e: e.matmul(
                sps[:, 0:QB], lhsT=KT[s][poff:poff + 64, kc, kt * 128:(kt + 1) * 128],
                rhs=QT[poff:poff + 64, qch, qb * QB:(qb + 1) * QB], start=True, stop=True),
                 reads=[b_KT[s], b_QT], writes=[bs])
            pb, bpb = Pb[si % 3], b_Pb[si % 3]
            R.op("act", lambda e: e.activation(out=pb[:, 0:QB], in_=sps[:, 0:QB], func=AF.Exp, scale=0.125),
                 reads=[bs], writes=[bpb])

        def emit_pv(st):
            kb, m, qb, u, kt = st
            s = kb % 2
            kc, poff, qch, vg = maps[m]
            si = u * 4 + kt
            pb, bpb = Pb[si % 3], b_Pb[si % 3]
            o_ps, b_o = ps_O[u % 2][:], bO[u % 2]
            for qc in range(NQC):
                R.op("pe", lambda e, qc=qc: e.matmul(
                    oreg(o_ps, qc), lhsT=pb[:, qc * 128:(qc + 1) * 128], rhs=Vt[s][:, kt, vg, 0:vd1],
                    start=(kt == 0 and (qc == 0 if vd1 * NQC <= 512 else qc % 2 == 0)), stop=(kt == 3),
                    skip_group_check=True), reads=[bpb, b_Vt[s]], writes=[b_o])
            if kt != 3:
                return
            qi0 = qb * NQC
            if vd1 * NQC <= 512:
                groups = [(0, NQC, o_ps[:, 0:NQC * vd1])]
            else:
                groups = [(bk * 2, 2, o_ps[:, bk * 512: bk * 512 + 2 * vd1]) for bk in range(NQC // 2)]
            for (q0, nq, src) in groups:
                dst = acc[:, m, qi0 + q0:qi0 + q0 + nq, :]
                srcv = src.rearrange("p (q v) -> p q v", v=vd1)
                if kb == 0:
                    R.op("dve", lambda e, dst=dst, srcv=srcv: e.tensor_copy(out=dst, in_=srcv),
                         reads=[b_o], writes=[b_acc])
                else:
                    R.op("dve", lambda e, dst=dst, srcv=srcv: e.tensor_tensor(out=dst, in0=dst, in1=srcv,
                                                                              op=ALU.add),
                         reads=[b_o, b_acc], writes=[b_acc])

        all_steps = []
        u = 0
        for kb in range(NKB):
            for m in range(nmap):
                for qb in range(NQB):
                    for kt in range(4):
                        all_steps.append((kb, m, qb, u, kt))
                    u += 1
        run(inproj(0))
        gen = None
        emit_qk(all_steps[0])
        for i, st in enumerate(all_steps):
            kb = st[0]
            if (i == 0 or all_steps[i - 1][0] != kb) and kb + 1 < NKB:
                gen = inproj(kb + 1)
            if i + 1 < len(all_steps):
                if all_steps[i + 1][0] != kb and gen is not None:
                    run(gen)
                    gen = None
                emit_qk(all_steps[i + 1])
            emit_pv(st)
            if gen is not None and i % 2 == 1:
                try:
                    next(gen)
                except StopIteration:
                    gen = None

        fin = A.alloc([nmap + 2 * nmap + 8], F32)
        b_fin = Buf(tag + "fin")
        if diff:
            o_t = A.alloc([4, 128], F32)
            o_u = A.alloc([4, 128], F32)
            b_ot = Buf(tag + "ot")
            b_ou = Buf(tag + "ou")
        for qi in range(NQI):
            rl = fin[:, 0:nmap]
            R.op("dve", lambda e, qi=qi: e.reciprocal(out=rl, in_=acc[:, :, qi, vdim]), reads=[b_acc], writes=[b_fin])
            if not diff:
                R.op("dve", lambda e, qi=qi: e.tensor_tensor(
                    out=Otok[:, qi, ocol0:ocol0 + nmap * vdim].rearrange("p (h v) -> p h v", v=vdim),
                    in0=acc[:, :, qi, 0:vdim], in1=rl.unsqueeze(2).to_broadcast([128, nmap, vdim]), op=ALU.mult),
                     reads=[b_acc, b_fin], writes=[b_Otok[qi]])
            else:
                rl3 = rl.rearrange("p (h c) -> p h c", c=2)
                rl1 = fin[:, nmap:nmap + 4]
                R.op("dve", lambda e: e.tensor_scalar(out=rl1, in0=rl3[:, :, 1], scalar1=lam, scalar2=None,
                                                      op0=ALU.mult), reads=[b_fin, b_small], writes=[b_fin])
                accv = acc.rearrange("p (h c) q v -> p h c q v", c=2)
                R.op("dve", lambda e, qi=qi: e.tensor_tensor(
                    out=o_t, in0=accv[:, :, 0, qi, 0:vdim],
                    in1=rl3[:, :, 0].unsqueeze(2).to_broadcast([128, 4, vdim]), op=ALU.mult),
                     reads=[b_acc, b_fin], writes=[b_ot])
                R.op("dve", lambda e, qi=qi: e.tensor_tensor(
                    out=o_u, in0=accv[:, :, 1, qi, 0:vdim],
                    in1=rl1.unsqueeze(2).to_broadcast([128, 4, vdim]), op=ALU.mult),
                     reads=[b_acc, b_fin], writes=[b_ou])
                R.op("dve", lambda e: e.tensor_tensor(out=o_t, in0=o_t, in1=o_u, op=ALU.subtract),
                     reads=[b_ot, b_ou], writes=[b_ot])
                R.op("dve", lambda e: e.tensor_tensor(out=o_u, in0=o_t, in1=o_t, op=ALU.mult),
                     reads=[b_ot], writes=[b_ou])
                ssq = fin[:, nmap + 4:nmap + 8]
                R.op("dve", lambda e: e.reduce_sum(out=ssq, in_=o_u, axis=AX.X), reads=[b_ou], writes=[b_fin])
                rsqrt_mean("dve", ssq, ssq, float(vdim), SUBLN_EPS, [b_fin], [b_fin])
                R.op("dve", lambda e: e.tensor_tensor(out=o_t, in0=o_t,
                                                      in1=ssq.unsqueeze(2).to_broadcast([128, 4, vdim]), op=ALU.mult),
                     reads=[b_ot, b_fin], writes=[b_ot])
                R.op("dve", lambda e, qi=qi: e.tensor_tensor(
                    out=Otok[:, qi, ocol0:ocol0 + 4 * vdim].rearrange("p (h v) -> p h v", v=vdim),
                    in0=o_t, in1=subg.unsqueeze(1).to_broadcast([128, 4, vdim]), op=ALU.mult),
                     reads=[b_ot, b_cst], writes=[b_Otok[qi]])

    import os
    stage = int(os.environ.get("KSTAGE", "3"))
    if stage < 3:
        for qi in range(NQI):
            R.op("pool", lambda e, qi=qi: e.memset(Otok[:, qi, :], 0.0), writes=[b_Otok[qi]])
    mapsA = [(0, (h // 4) * 64, h % 4, h // 4) for h in range(8)]
    if stage >= 2:
        attn_phase("A", d_wA, 512, 1, 64, 2, True, d_tabA, d_tabAo, permA, mapsA, 0, False)
    mapsB = [(hh, comp * 64, hh, hh) for hh in range(4) for comp in range(2)]
    if stage >= 3:
        attn_phase("B", d_wB, 512, 4, 128, 4, False, d_tabB, d_tabBo, permB, mapsB, 512, True)

    A.off = phase_mark
    R.barrier()
    xn2T = A.alloc([8, TQ], BF16)
    b_xn2T = Buf("xn2T")
    f1_mark = A.off
    gffn = A.alloc([D], F32)
    b_gffn = Buf("gffn")
    R.dma("sp", lambda e: e.dma_start(out=gffn, in_=d_gffn), b_gffn, writes=[b_gffn])
    wout = A.alloc([8, D], BF16)
    b_wout = Buf("wout")
    wst = A.alloc([512], F32)
    b_wst = Buf("wst2")
    for c in range(8):
        for c0 in range(0, D, 512):
            R.dma("sp", lambda e, c=c, c0=c0: e.dma_start(out=wst, in_=d_wout[:, c, c0:c0 + 512]), b_wst,
                  writes=[b_wst])
            R.op("pool", lambda e, c=c, c0=c0: e.tensor_copy(out=wout[:, c, c0:c0 + 512], in_=wst),
                 reads=[b_wst], writes=[b_wout])
    OT = A.alloc([8, 128], BF16)
    b_OT = Buf("OT")
    xo = A.alloc([D], F32)
    b_xo = Buf("xo")
    hbuf = A.alloc([D], F32)
    b_h = Buf("h")
    junk = A.alloc([D], F32)
    b_junk = Buf("junk")
    obuf = A.alloc([D], F32)
    b_ob = Buf("obuf")
    xn2 = A.alloc([D], BF16)
    b_xn2 = Buf("xn2")
    st = A.alloc([8], F32)
    b_st = Buf("st")
    b_hs = [Buf("hs%d" % i) for i in range(NQI)]

    def final_norm(src, b_src, junk_, b_junk_, st_, b_st_, dst, b_dst, qi):
        R.op("act", lambda e: e.activation(out=junk_, in_=src, func=AF.Square, accum_out=st_[:, 0:1]),
             reads=[b_src], writes=[b_junk_, b_st_])
        rsqrt_mean("dve", st_[:, 1:2], st_[:, 0:1], float(D), NORM_EPS, [b_st_], [b_st_])
        R.op("dve", lambda e: e.scalar_tensor_tensor(out=dst, in0=src, scalar=st_[:, 1:2], in1=gfin,
                                                     op0=ALU.mult, op1=ALU.mult),
             reads=[b_src, b_st_, b_cst], writes=[b_dst])
        R.dma("sp", lambda e: e.dma_start(out=d_out[qi * 128:(qi + 1) * 128, :], in_=dst), b_dst, reads=[b_dst])

    def f1_tile(qi):
        R.dma("sp", lambda e: e.dma_start(out=xo, in_=d_xo[qi * 128:(qi + 1) * 128, :]), b_xo, writes=[b_xo])
        tp, btp = next_pp()
        tpb = tp.bitcast(BF16)
        for c in range(8):
            R.op("pe", lambda e, c=c: e.transpose(tpb[:, c * 128:(c + 1) * 128],
                                                  Otok[:, qi, c * 128:(c + 1) * 128], ident),
                 reads=[b_Otok[qi], b_cbf], writes=[btp])
        R.op("act", lambda e: e.activation(out=OT, in_=tpb.rearrange("p (c q) -> p c q", q=128), func=AF.Copy),
             reads=[btp], writes=[b_OT])
        for hf in range(2):
            ps, bps = next_pp()
            for c in range(8):
                R.op("pe", lambda e, c=c, hf=hf, ps=ps: e.matmul(ps, lhsT=OT[:, c, :],
                                                                 rhs=wout[:, c, hf * 512:(hf + 1) * 512],
                                                                 start=(c == 0), stop=(c == 7)),
                     reads=[b_OT, b_wout], writes=[bps])
            R.op("dve", lambda e, hf=hf, ps=ps: e.tensor_tensor(out=hbuf[:, hf * 512:(hf + 1) * 512], in0=ps,
                                                                in1=xo[:, hf * 512:(hf + 1) * 512], op=ALU.add),
                 reads=[bps, b_xo], writes=[b_h])
        if not with_peer:
            final_norm(hbuf, b_h, junk, b_junk, st, b_st, obuf, b_ob, qi)
            return
        R.dma("sp", lambda e: e.dma_start(out=d_hs[qi * 128:(qi + 1) * 128, :], in_=hbuf), b_h,
              reads=[b_h], writes=[b_hs[qi]])
        R.op("act", lambda e: e.activation(out=junk, in_=hbuf, func=AF.Square, accum_out=st[:, 0:1]),
             reads=[b_h], writes=[b_junk, b_st])
        rsqrt_mean("dve", st[:, 1:2], st[:, 0:1], float(D), NORM_EPS, [b_st], [b_st])
        R.op("dve", lambda e: e.scalar_tensor_tensor(out=xn2, in0=hbuf, scalar=st[:, 1:2], in1=gffn,
                                                     op0=ALU.mult, op1=ALU.mult),
             reads=[b_h, b_st, b_gffn], writes=[b_xn2])
        tp2, btp2 = next_pp()
        tpb2 = tp2.bitcast(BF16)
        for c in range(8):
            R.op("pe", lambda e, c=c: e.transpose(tpb2[:, c * 128:(c + 1) * 128], xn2[:, c * 128:(c + 1) * 128],
                                                  ident), reads=[b_xn2, b_cbf], writes=[btp2])
        R.op("act", lambda e: e.activation(out=xn2T[:, :, qi * 128:(qi + 1) * 128],
                                           in_=tpb2.rearrange("p (c q) -> p c q", q=128), func=AF.Copy),
             reads=[btp2], writes=[b_xn2T])

    for qi in range(NQI):
        f1_tile(qi)

    if with_peer:
        A.off = f1_mark
        A.n = ARENA_WORDS
        R.barrier()
        TB = min(4, NQI)
        NBK = NQI // TB
        NT = TB * 128
        NEG = -1.0e30
        skT = A.alloc([16, 128], BF16)
        b_skT = Buf("skT")
        sel = [A.alloc([16, 128], F32) for _ in range(TB)]
        b_sel = [Buf("sel%d" % i) for i in range(TB)]
        ssm = [A.alloc([64], F32) for _ in range(TB)]
        b_ssm = [Buf("ssm%d" % i) for i in range(TB)]
        Dg = [A.alloc([8, 128], BF16) for _ in range(TB)]
        b_Dg = [Buf("Dg%d" % i) for i in range(TB)]
        oacc = A.alloc([TB, D], F32)
        b_oacc = [Buf("oacc%d" % i) for i in range(TB)]
        st2 = A.alloc([8], F32)
        b_st2 = Buf("st2")
        union_mark = A.off
        qpT = A.alloc([16, NT], BF16)
        b_qpT = Buf("qpT")
        sc = A.alloc([16, 128], F32)
        b_sc = Buf("sc")
        cand = A.alloc([8, 256], F32)
        b_cand = Buf("cand")
        wqs = A.alloc([8, 128], F32)
        b_wqs = Buf("wqs")
        wqb = A.alloc([8, 128], BF16)
        b_wqb = Buf("wqb")
        vtop = A.alloc([16, 16], F32)
        b_vtop = Buf("vtop")
        top = A.alloc([8, 24], F32)
        b_top = Buf("top")
        mt16 = A.alloc([16, 128], F32)
        b_mt = [Buf("mt%d" % i) for i in range(16)]
        b_vt = [Buf("vt%d" % i) for i in range(16)]
        b_tp = [Buf("tp%d" % i) for i in range(8)]
        d16 = A.alloc([8, 16], F32)
        b_d16 = Buf("d16")
        A.off = union_mark
        hb2 = A.alloc([D], F32)
        b_hb2 = Buf("hb2")
        jk2 = A.alloc([D], F32)
        b_jk2 = Buf("jk2")
        ob2 = A.alloc([D], F32)
        b_ob2 = Buf("ob2")
        A.off = union_mark
        ust = A.alloc([8, 512], F32)
        b_ust = Buf("ust")
        vst = A.alloc([4, D], F32)
        b_vst = Buf("vst")
        ubf = [A.alloc([8, 512], BF16) for _ in range(2)]
        b_ubf = [Buf("ubf%d" % i) for i in range(2)]
        vbf = [A.alloc([4, D], BF16) for _ in range(2)]
        b_vbf = [Buf("vbf%d" % i) for i in range(2)]
        Eb = [A.alloc([4, 512], F32) for _ in range(2)]
        b_Eb = [Buf("Eb%d" % i) for i in range(2)]
        Mb = A.alloc([8, 512], BF16)
        b_Mb = Buf("Mb")
        gaT = [A.alloc([4, NT], BF16) for _ in range(2)]
        b_gaT = [Buf("gaT%d" % i) for i in range(2)]
        HT = [A.alloc([4, 128], BF16) for _ in range(2)]
        b_HT = [Buf("HT%d" % i) for i in range(2)]
        ytmp = A.alloc([D], F32)
        b_ytmp = Buf("ytmp")

        skst = vst[:, 0:2, :].rearrange("p a (h n) -> p (a h) n", n=128)
        R.dma("sp", lambda e: e.dma_start(out=skst, in_=d_skT), b_vst, writes=[b_vst])
        R.op("dve", lambda e: e.tensor_copy(out=skT, in_=skst), reads=[b_vst], writes=[b_skT])
        uid = [0]

        def select_block(bk):
            t0 = bk * NT
            for hp in range(16):
                R.dma("sp", lambda e, hp=hp: e.dma_start(out=wqs, in_=d_wq[:, :, hp * 128:(hp + 1) * 128]), b_wqs,
                      writes=[b_wqs])
                R.op("pool", lambda e: e.tensor_copy(out=wqb, in_=wqs), reads=[b_wqs], writes=[b_wqb])
                ps, bps = next_pp()
                for c in range(8):
                    R.op("pe", lambda e, c=c, ps=ps: e.matmul(ps[:, 0:NT], lhsT=wqb[:, c, :],
                                                              rhs=xn2T[:, c, t0:t0 + NT], start=(c == 0),
                                                              stop=(c == 7)), reads=[b_wqb, b_xn2T], writes=[bps])
                R.op("act", lambda e, hp=hp, ps=ps: e.activation(out=qpT[:, hp, :], in_=ps[:, 0:NT], func=AF.Copy),
                     reads=[bps], writes=[b_qpT])
            for tt in range(TB):
                select_tile(tt)

        def select_tile(tt):
            sl, bsl, sm, bsm = sel[tt], b_sel[tt], ssm[tt], b_ssm[tt]
            for g in range(4):
                ps, bps = next_pp()
                for i in range(4):
                    hp = g * 4 + i
                    R.op("pe", lambda e, i=i, hp=hp, ps=ps: e.matmul(
                        ps[:, i * 128:(i + 1) * 128], lhsT=qpT[:, hp, tt * 128:(tt + 1) * 128], rhs=skT[:, hp, :],
                        start=True, stop=True, skip_group_check=True), reads=[b_qpT, b_skT], writes=[bps])
                R.op("act", lambda e, g=g, ps=ps: e.activation(
                    out=sc[:, g * 4:(g + 1) * 4, :], in_=ps.rearrange("p (a n) -> p a n", n=128), func=AF.Copy),
                     reads=[bps], writes=[b_sc])
            for hp in range(16):
                R.op("dve", lambda e, hp=hp: e.max(out=vtop[:, hp, 0:8], in_=sc[:, hp, :]), reads=[b_sc],
                     writes=[b_vt[hp]])
            for hp in range(16):
                R.op("dve", lambda e, hp=hp: e.match_replace(out=mt16[:, hp, :], in_to_replace=vtop[:, hp, 0:8],
                                                             in_values=sc[:, hp, :], imm_value=NEG),
                     reads=[b_sc, b_vt[hp]], writes=[b_mt[hp]])
            for hp in range(16):
                R.op("dve", lambda e, hp=hp: e.max(out=vtop[:, hp, 8:16], in_=mt16[:, hp, :]), reads=[b_mt[hp]],
                     writes=[b_vt[hp]])
            vv = vtop.rearrange("p (h two) k -> p h two k", two=2)
            cand4 = cand.rearrange("p h (i j) -> p h i j", j=16)
            R.op("dve", lambda e: e.tensor_tensor(
                out=cand4, in0=vv[:, :, 0, :].unsqueeze(3).to_broadcast([128, 8, 16, 16]),
                in1=vv[:, :, 1, :].unsqueeze(2).to_broadcast([128, 8, 16, 16]), op=ALU.add),
                 reads=b_vt, writes=[b_cand])
            mtc = mt16.rearrange("p (h two) n -> p h (two n)", two=2)
            for h in range(8):
                R.op("dve", lambda e, h=h: e.max(out=top[:, h, 0:8], in_=cand[:, h, :]), reads=[b_cand],
                     writes=[b_tp[h]])
            for h in range(8):
                R.op("dve", lambda e, h=h: e.match_replace(out=mtc[:, h, :], in_to_replace=top[:, h, 0:8],
                                                           in_values=cand[:, h, :], imm_value=NEG),
                     reads=[b_cand, b_tp[h]], writes=[b_mt[2 * h], b_mt[2 * h + 1]])
            for h in range(8):
                R.op("dve", lambda e, h=h: e.max(out=top[:, h, 8:16], in_=mtc[:, h, :]),
                     reads=[b_mt[2 * h], b_mt[2 * h + 1]], writes=[b_tp[h]])
            for h in range(8):
                R.op("dve", lambda e, h=h: e.match_replace(out=mtc[:, h, :], in_to_replace=top[:, h, 8:16],
                                                           in_values=mtc[:, h, :], imm_value=NEG),
                     reads=[b_mt[2 * h], b_mt[2 * h + 1], b_tp[h]], writes=[b_mt[2 * h], b_mt[2 * h + 1]])
            for h in range(8):
                R.op("dve", lambda e, h=h: e.max(out=top[:, h, 16:24], in_=mtc[:, h, :]),
                     reads=[b_mt[2 * h], b_mt[2 * h + 1]], writes=[b_tp[h]])
            b_top_all = b_tp
            mx = top[:, :, 0]
            R.op("dve", lambda e: e.tensor_tensor(out=d16, in0=top[:, :, 0:16],
                                                  in1=top[:, :, 0:1].to_broadcast([128, 8, 16]), op=ALU.subtract),
                 reads=b_tp, writes=[b_d16])
            R.op("act", lambda e: e.activation(out=d16, in_=d16, func=AF.Exp), reads=[b_d16], writes=[b_d16])
            R.op("dve", lambda e: e.reduce_sum(out=sm[:, 8:16], in_=d16, axis=AX.X), reads=[b_d16], writes=[bsm])
            R.op("dve", lambda e: e.reciprocal(out=sm[:, 8:16], in_=sm[:, 8:16]), reads=[bsm], writes=[bsm])
            R.op("dve", lambda e: e.tensor_tensor(out=sm[:, 16:24], in0=top[:, :, 15], in1=top[:, :, 16], op=ALU.add),
                 reads=b_tp, writes=[bsm])
            R.op("dve", lambda e: e.scalar_tensor_tensor(out=sm[:, 16:24], in0=sm[:, 16:24], scalar=0.5, in1=mx,
                                                         op0=ALU.mult, op1=ALU.subtract),
                 reads=[bsm] + b_tp, writes=[bsm])
            R.op("act", lambda e: e.activation(out=sm[:, 16:24], in_=sm[:, 16:24], func=AF.Exp), reads=[bsm],
                 writes=[bsm])
            R.op("dve", lambda e: e.tensor_tensor(out=sm[:, 0:8], in0=sm[:, 16:24], in1=sm[:, 8:16], op=ALU.mult),
                 reads=[bsm], writes=[bsm])
            R.op("dve", lambda e: e.reciprocal(out=sm[:, 24:32], in_=sm[:, 16:24]), reads=[bsm], writes=[bsm])
            R.op("dve", lambda e: e.tensor_tensor(out=sl, in0=sc, in1=vtop[:, :, 0:1].to_broadcast([128, 16, 128]),
                                                  op=ALU.subtract), reads=[b_sc] + b_vt, writes=[bsl])
            R.op("act", lambda e: e.activation(out=sl, in_=sl, func=AF.Exp), reads=[bsl], writes=[bsl])
            slv = sl.rearrange("p (h two) n -> p h two n", two=2)
            R.op("dve", lambda e: e.tensor_tensor(out=slv[:, :, 0, :], in0=slv[:, :, 0, :],
                                                  in1=sm[:, 24:32].unsqueeze(2).to_broadcast([128, 8, 128]),
                                                  op=ALU.mult), reads=[bsl, bsm], writes=[bsl])
            for h in range(8):
                R.op("dve", lambda e, h=h: e.tensor_scalar(out=Dg[tt][:, h, :], in0=ident, scalar1=sm[:, h:h + 1],
                                                           scalar2=None, op0=ALU.mult),
                     reads=[b_cbf, bsm], writes=[b_Dg[tt]])

        def load_chunk(j):
            k = j % 2
            R.dma("sp", lambda e: e.dma_start(out=ust, in_=d_uT[:, :, j * 512:(j + 1) * 512]), b_ust, writes=[b_ust])
            R.op("act", lambda e: e.activation(out=ubf[k], in_=ust, func=AF.Copy), reads=[b_ust], writes=[b_ubf[k]])
            R.dma("sp", lambda e: e.dma_start(
                out=vst, in_=d_ev[j * 512:(j + 1) * 512, :].rearrange("(a p) d -> p a d", p=128)), b_vst,
                  writes=[b_vst])
            R.op("act", lambda e: e.activation(out=vbf[k], in_=vst, func=AF.Copy), reads=[b_vst], writes=[b_vbf[k]])

        def chunk_gelu(bk, j):
            k = j % 2
            t0 = bk * NT
            for a in range(4):
                u = uid[0]
                uid[0] += 1
                psA, bA = ps_S[u % 2][:], bS[u % 2]
                for c in range(8):
                    R.op("pe", lambda e, c=c, a=a, psA=psA: e.matmul(
                        psA[:, 0:NT], lhsT=ubf[k][:, c, a * 128:(a + 1) * 128], rhs=xn2T[:, c, t0:t0 + NT],
                        start=(c == 0), stop=(c == 7)), reads=[b_ubf[k], b_xn2T], writes=[bA])
                R.op("act", lambda e, a=a, psA=psA: e.activation(out=gaT[k][:, a, :], in_=psA[:, 0:NT], func=AF.Gelu),
                     reads=[bA], writes=[b_gaT[k]])

        b_Mbh = [Buf("Mb%d" % i) for i in range(2)]
        b_Eb1a, b_Eb1b = Buf("Eb1a"), Buf("Eb1b")

        def gates_E(tt, j):
            slv = sel[tt].rearrange("p (h two) n -> p h two n", two=2)
            R.op("dve", lambda e: e.tensor_tensor(
                out=Eb[0].rearrange("p h (a n) -> p h a n", n=128),
                in0=slv[:, 0:4, 0, 4 * j:4 * j + 4].unsqueeze(3).to_broadcast([128, 4, 4, 128]),
                in1=slv[:, 0:4, 1, :].unsqueeze(2).to_broadcast([128, 4, 4, 128]), op=ALU.mult),
                 reads=[b_sel[tt]], writes=[b_Eb[0]])
            E1v = Eb[1].rearrange("p h (a n) -> p h a n", n=128)
            R.op("dve", lambda e: e.tensor_tensor(
                out=E1v[:, 0:2],
                in0=slv[:, 4:6, 0, 4 * j:4 * j + 4].unsqueeze(3).to_broadcast([128, 2, 4, 128]),
                in1=slv[:, 4:6, 1, :].unsqueeze(2).to_broadcast([128, 2, 4, 128]), op=ALU.mult),
                 reads=[b_sel[tt]], writes=[b_Eb1a])
            for h in range(2, 4):
                for a in range(4):
                    R.op("act", lambda e, h=h, a=a: e.activation(
                        out=E1v[:, h, a, :], in_=slv[:, 4 + h, 1, :], func=AF.Copy,
                        scale=slv[:, 4 + h, 0, 4 * j + a:4 * j + a + 1]), reads=[b_sel[tt]], writes=[b_Eb1b])

        def gates_M(tt, j):
            for hh in range(2):
                R.op("dve", lambda e, hh=hh: e.scalar_tensor_tensor(
                    out=Mb[:, hh * 4:(hh + 1) * 4, :], in0=Eb[hh], scalar=1.0, in1=Eb[hh], op0=ALU.is_ge,
                    op1=ALU.mult), reads=([b_Eb[0]] if hh == 0 else [b_Eb1a, b_Eb1b]), writes=[b_Mbh[hh]])

        def transposes(tt, j):
            gp, bgp = next_pp()
            for h in range(8):
                for a in range(4):
                    R.op("pe", lambda e, a=a, h=h: e.matmul(
                        gp[:, a * 128:(a + 1) * 128], lhsT=Mb[:, h, a * 128:(a + 1) * 128], rhs=Dg[tt][:, h, :],
                        start=(h == 0 and a == 0), stop=(h == 7), skip_group_check=True),
                         reads=[b_Mbh[h // 4], b_Dg[tt]], writes=[bgp])
            return gp, bgp

        def make_HT(tt, j, u, gp, bgp):
            k = j % 2
            htu, bhtu = HT[u % 2], b_HT[u % 2]
            R.op("dve", lambda e: e.tensor_tensor(out=htu, in0=gp.rearrange("p (a t) -> p a t", t=128),
                                                  in1=gaT[k][:, :, tt * 128:(tt + 1) * 128], op=ALU.mult),
                 reads=[bgp, b_gaT[k]], writes=[bhtu])

        def final(tt, j, u):
            k = j % 2
            htu, bhtu = HT[u % 2], b_HT[u % 2]
            psY, bY = ps_O[u % 2][:], bO[u % 2]
            for hf in range(2):
                for a in range(4):
                    R.op("pe", lambda e, a=a, hf=hf: e.matmul(
                        psY[:, hf * 512:(hf + 1) * 512], lhsT=htu[:, a, :], rhs=vbf[k][:, a, hf * 512:(hf + 1) * 512],
                        start=(a == 0), stop=(a == 3)), reads=[bhtu, b_vbf[k]], writes=[bY])
            if j == 0:
                R.op("act", lambda e: e.activation(out=oacc[:, tt, :], in_=psY, func=AF.Copy), reads=[bY],
                     writes=[b_oacc[tt]])
            else:
                R.op("act", lambda e: e.activation(out=ytmp, in_=psY, func=AF.Copy), reads=[bY], writes=[b_ytmp])
                R.op("pool", lambda e: e.tensor_tensor(out=oacc[:, tt, :], in0=oacc[:, tt, :], in1=ytmp, op=ALU.add),
                     reads=[b_ytmp, b_oacc[tt]], writes=[b_oacc[tt]])

        NCH = NE // 512
        for bk in range(NBK):
            R.barrier()
            select_block(bk)
            R.barrier()
            load_chunk(0)
            chunk_gelu(bk, 0)
            ulist = [(j, tt) for j in range(NCH) for tt in range(TB)]
            gates_E(ulist[0][1], ulist[0][0])
            gates_M(ulist[0][1], ulist[0][0])
            for i, (j, tt) in enumerate(ulist):
                u = uid[0]
                uid[0] += 1
                if tt == 0 and j + 1 < NCH:
                    load_chunk(j + 1)
                gp, bgp = transposes(tt, j)
                nxt = ulist[i + 1] if i + 1 < len(ulist) else None
                if nxt is not None:
                    gates_E(nxt[1], nxt[0])
                make_HT(tt, j, u, gp, bgp)
                if nxt is not None:
                    gates_M(nxt[1], nxt[0])
                final(tt, j, u)
                if tt == min(1, TB - 1) and j + 1 < NCH:
                    chunk_gelu(bk, j + 1)
            R.barrier()
            for tt in range(TB):
                qi = bk * TB + tt

                def epilogue(tt=tt, qi=qi):
                    R.dma("sp", lambda e: e.dma_start(out=hb2, in_=d_hs[qi * 128:(qi + 1) * 128, :]), b_hb2,
                          reads=[b_hs[qi]], writes=[b_hb2])
                    R.op("dve", lambda e: e.tensor_tensor(out=hb2, in0=hb2, in1=oacc[:, tt, :], op=ALU.add),
                         reads=[b_hb2, b_oacc[tt]], writes=[b_hb2])
                    final_norm(hb2, b_hb2, jk2, b_jk2, st2, b_st2, ob2, b_ob2, qi)
                epilogue()

    counts = R.finish(nc, stack)
    stack.close()
    print('[kernel] arena peak words', A.peak, 'of', ARENA_WORDS, 'sem incs', counts)
    return nc, counts, A.peak


def _chunk_rows(w):
    n = w.shape[1]
    return np.ascontiguousarray(w.reshape(8, 128, n).transpose(1, 0, 2))


def _tables(S):
    f32 = np.float32
    t = np.arange(S)
    row = (t // 64).astype(f32)
    col = (t % 64).astype(f32)
    pos = t.astype(f32)
    inv_ax = (f32(10000.0) ** (-np.arange(0, 32, 2, dtype=f32) / f32(32))).astype(f32)
    inv_p = (f32(500000.0) ** (-np.arange(0, 16, 2, dtype=f32) / f32(16))).astype(f32)
    ra = (row[:, None] * inv_ax[None, :]).astype(f32)
    ca = (col[:, None] * inv_ax[None, :]).astype(f32)
    pa = (pos[:, None] * inv_p[None, :]).astype(f32)
    cA = np.concatenate([np.cos(ra), np.cos(ra), np.cos(ca), np.cos(ca)], axis=1).T
    sA = np.concatenate([-np.sin(ra), np.sin(ra), -np.sin(ca), np.sin(ca)], axis=1).T
    cB = np.concatenate([np.cos(pa), np.cos(pa), np.ones((S, 48), f32)], axis=1).T
    sB = np.concatenate([-np.sin(pa), np.sin(pa), np.zeros((S, 48), f32)], axis=1).T
    tabA = np.stack([np.tile(cA, (2, 1)), np.tile(sA, (2, 1))], axis=1).astype(f32)
    tabB = np.stack([np.tile(cB, (2, 1)), np.tile(sB, (2, 1))], axis=1).astype(f32)
    return np.ascontiguousarray(tabA), np.ascontiguousarray(tabB)


def _consts():
    ident = np.eye(128, dtype=np.float32)
    ones = np.ones((128, 128), np.float32)
    blk = np.zeros((128, 128), np.float32)
    blk[:64, :64] = 1
    blk[64:, 64:] = 1

    def perm(partner):
        p = np.zeros((128, 128), np.float32)
        for m in range(128):
            p[(m // 64) * 64 + partner(m % 64), m] = 1
        return p

    def pa(d):
        return d + 16 if (d % 32) < 16 else d - 16

    def pb(d):
        if d < 8:
            return d + 8
        if d < 16:
            return d - 8
        return d

    return np.ascontiguousarray(np.stack([ident, ones, blk, perm(pa), perm(pb)], axis=1))


_CACHE = {}


def kernel(x, norm_attn_g, w_in, q_norm_g, k_norm_g, lambda_q1, lambda_k1, lambda_q2, lambda_k2, subln_g,
           w_out, norm_ffn_g, w_query, sub_keys, expert_u, expert_v, norm_final_g, _with_peer=True):
    f32 = np.float32
    x = np.asarray(x, f32)
    S = x.shape[1]
    TQ = S // NCORE
    x2 = x[0]
    xT = np.ascontiguousarray(x2.T.reshape(8, 128, S).transpose(1, 0, 2))
    w = np.asarray(w_in, f32)[0]
    qa, ka, va = w[:, 0:512], w[:, 512:640], w[:, 640:768]
    qb, kb, vb = w[:, 768:1280], w[:, 1280:1792], w[:, 1792:2304]
    qa_p = np.concatenate([np.concatenate([qa[:, j * 64:(j + 1) * 64], qa[:, (j + 4) * 64:(j + 5) * 64]], axis=1)
                           for j in range(4)], axis=1)
    wA = _chunk_rows(np.concatenate([qa_p, ka, va], axis=1))
    wB = _chunk_rows(np.concatenate([qb, kb, vb], axis=1))
    wout = _chunk_rows(np.asarray(w_out, f32)[0])
    tabA, tabB = _tables(S)
    vec = np.zeros((128, 18), f32)
    vec[:, 0:8] = np.asarray(norm_attn_g, f32)[0].reshape(8, 128).T
    vec[:, 8:16] = np.asarray(norm_ffn_g, f32)[0].reshape(8, 128).T
    vec[:, 16] = np.tile(np.asarray(q_norm_g, f32)[0], 2)
    vec[:, 17] = np.tile(np.asarray(k_norm_g, f32)[0], 2)
    lam = np.stack([np.asarray(a, f32)[0] for a in (lambda_q1, lambda_k1, lambda_q2, lambda_k2)], axis=0)
    lam = np.ascontiguousarray(np.broadcast_to(lam[None], (128, 4, 64)))
    sub = np.ascontiguousarray(np.broadcast_to(np.asarray(subln_g, f32)[0][None], (128, 128)))
    gfin = np.ascontiguousarray(np.broadcast_to(np.asarray(norm_final_g, f32)[None], (128, D)))
    gffn = np.ascontiguousarray(np.broadcast_to(np.asarray(norm_ffn_g, f32)[0][None], (128, D)))
    cst = _consts()
    key = (S, _with_peer)
    if key not in _CACHE:
        _CACHE[key] = build(S, _with_peer)
    nc, counts, peak = _CACHE[key]
    common = dict(xT=xT, wA=wA, wB=wB, wout=wout, cst=cst, tabA=tabA, tabB=tabB, vec=vec, lam=lam, subln=sub,
                  gfin=gfin, gffn=gffn)
    if _with_peer:
        common["wq"] = _chunk_rows(np.asarray(w_query, f32)[0])
        sk = np.asarray(sub_keys, f32)[0].reshape(16, 128, 128)
        common["skT"] = np.ascontiguousarray(sk.transpose(2, 0, 1))
        common["uT"] = _chunk_rows(np.ascontiguousarray(np.asarray(expert_u, f32)[0].T))
        common["ev"] = np.ascontiguousarray(np.asarray(expert_v, f32)[0])
    in_maps = []
    for c in range(NCORE):
        sl = slice(c * TQ, (c + 1) * TQ)
        m = dict(common)
        m["xTo"] = np.ascontiguousarray(xT[:, :, sl])
        m["xo"] = np.ascontiguousarray(x2[sl])
        m["tabAo"] = np.ascontiguousarray(tabA[:, :, sl])
        m["tabBo"] = np.ascontiguousarray(tabB[:, :, sl])
        in_maps.append(m)
    res = run_bass_kernel_spmd(nc, in_maps, core_ids=list(range(NCORE)))
    out = np.concatenate([np.asarray(r["out"], f32) for r in res.results], axis=0)
    return out.reshape(1, S, D)
```
